# Optimizing a Trainium2 kernel written in Bass

```python
import math
import jax, jax.numpy as jnp
from jax import lax
import numpy as np

D_MODEL = 1024
BATCH = 8
SEQ = 4096
DEPTH = 1

GRID_W = 64
CTX_LEN = 256
N_MOD = 6
EPS = 1e-6

F_GROUPS = 8
F_GROUP_DIM = 128
F_WIDTH = F_GROUPS * F_GROUP_DIM

D_INNER = 2 * D_MODEL
HEAD_DIM = 64
N_HEADS = D_INNER // HEAD_DIM
N_GROUPS = 4
HEADS_PER_GROUP = N_HEADS // N_GROUPS
D_STATE = 128
BC_WIDTH = N_GROUPS * D_STATE
CONV_K = 5
CHUNK = 128

N_EXPERTS = 32
TOP_K = 4
D_FF = D_MODEL
SWIGLU_LIMIT = 7.0
SWIGLU_ALPHA = 1.702
MOE_BLOCK = 128

OFF_X = 0
OFF_BF = OFF_X + D_INNER
OFF_BB = OFF_BF + BC_WIDTH
OFF_DT = OFF_BB + BC_WIDTH
OFF_CF = OFF_DT + 2 * N_HEADS
OFF_CB = OFF_CF + BC_WIDTH
OFF_Z = OFF_CB + BC_WIDTH
OFF_F = OFF_Z + D_INNER
OFF_GF = OFF_F + F_WIDTH
OFF_GS = OFF_GF + D_MODEL
IN_COLS = OFF_GS + D_MODEL
XB_CH = OFF_DT
CONV_CH = XB_CH + 2 * BC_WIDTH

kernel_name = 'hybrid_fnet_ssd_moe_prefix_dit_block'


def rmsnorm(v, w):
    v32 = v.astype(jnp.float32)
    inv = lax.rsqrt(jnp.mean(v32 * v32, axis=-1, keepdims=True) + EPS)
    return (v32 * inv * w.astype(jnp.float32)).astype(v.dtype)


def dwconv_centred(u, w, bias):
    ch = u.shape[-1]
    y = lax.conv_general_dilated(u, w[:, None, :].astype(u.dtype), window_strides=(1,),
                                 padding=[(CONV_K // 2, CONV_K // 2)],
                                 dimension_numbers=('NWC', 'WIO', 'NWC'),
                                 feature_group_count=ch)
    return y + bias


def _decay_matrix(a_cs):
    q = a_cs.shape[-1]
    mask = jnp.tril(jnp.ones((q, q), dtype=bool))
    diff = a_cs[..., :, None] - a_cs[..., None, :]
    return jnp.exp(jnp.where(mask, diff, -jnp.inf))


def _ssd_chunks(x, dt, a, b_in):
    bsz, l = x.shape[:2]
    nc = l // CHUNK
    a_cs = jnp.cumsum((dt * a).reshape(bsz, nc, CHUNK, N_GROUPS, HEADS_PER_GROUP), axis=2)
    xdt = (x.astype(jnp.float32) * dt[..., None]).reshape(bsz, nc, CHUNK, N_GROUPS, HEADS_PER_GROUP, HEAD_DIM)
    bc = b_in.astype(jnp.float32).reshape(bsz, nc, CHUNK, N_GROUPS, D_STATE)
    decay_to_end = jnp.exp(a_cs[:, :, -1:] - a_cs)
    states = jnp.einsum('bcsgn,bcsgh,bcsghp->bcghpn', bc, decay_to_end, xdt)
    chunk_decay = jnp.exp(a_cs[:, :, -1])
    return a_cs, xdt, bc, states, chunk_decay


def _carry_states(states, chunk_decay, h0):
    def step(h, inp):
        s, dec = inp
        return h * dec[..., None, None] + s, h
    h_final, h_prev = lax.scan(step, h0, (jnp.moveaxis(states, 1, 0), jnp.moveaxis(chunk_decay, 1, 0)))
    return jnp.moveaxis(h_prev, 0, 1), h_final


def ssd_final_state(x, dt, a, b_in, h0):
    _, _, _, states, chunk_decay = _ssd_chunks(x, dt, a, b_in)
    return _carry_states(states, chunk_decay, h0)[1]


def ssd_scan(x, dt, a, b_in, c_in, h0):
    bsz, l = x.shape[:2]
    nc = l // CHUNK
    a_cs, xdt, bc, states, chunk_decay = _ssd_chunks(x, dt, a, b_in)
    h_prev, _ = _carry_states(states, chunk_decay, h0)
    cc = c_in.astype(jnp.float32).reshape(bsz, nc, CHUNK, N_GROUPS, D_STATE)
    decay_in = _decay_matrix(jnp.moveaxis(a_cs, 2, -1))
    cb = jnp.einsum('bclgn,bcsgn->bcgls', cc, bc)
    y_diag = jnp.einsum('bcgls,bcghls,bcsghp->bclghp', cb, decay_in, xdt)
    y_off = jnp.einsum('bclgn,bcghpn,bclgh->bclghp', cc, h_prev, jnp.exp(a_cs))
    return (y_diag + y_off).reshape(bsz, l, N_HEADS, HEAD_DIM)


def moe_ffn(h, w_router, b_router, w_gate_up, b_gate_up, w_down, b_down):
    bsz, n, d = h.shape
    tok = h.reshape(-1, d)
    n_tok = tok.shape[0]
    logits = (tok @ w_router + b_router).astype(jnp.float32)
    top_logit, top_idx = lax.top_k(logits, TOP_K)
    top_w = jax.nn.softmax(top_logit, axis=-1)
    flat_e = top_idx.reshape(-1)
    flat_w = top_w.reshape(-1)
    n_assign = flat_e.shape[0]
    order = jnp.argsort(flat_e)
    e_sorted = flat_e[order]
    tok_sorted = (order // TOP_K).astype(jnp.int32)
    counts = jnp.bincount(flat_e, length=N_EXPERTS)
    padded = (counts + MOE_BLOCK - 1) // MOE_BLOCK * MOE_BLOCK
    pad_end = jnp.cumsum(padded)
    pad_start = pad_end - padded
    start = jnp.cumsum(counts) - counts
    dest = pad_start[e_sorted] + jnp.arange(n_assign) - start[e_sorted]
    n_blocks = (n_assign + MOE_BLOCK - 1) // MOE_BLOCK + N_EXPERTS
    rows = n_blocks * MOE_BLOCK
    row_tok = jnp.full((rows,), n_tok, jnp.int32).at[dest].set(tok_sorted)
    row_w = jnp.zeros((rows,), jnp.float32).at[dest].set(flat_w[order])
    blk_e = jnp.minimum(jnp.searchsorted(pad_end, jnp.arange(n_blocks) * MOE_BLOCK, side='right'),
                        N_EXPERTS - 1)
    tok_ext = jnp.concatenate([tok, jnp.zeros((1, d), tok.dtype)], axis=0)
    xb = tok_ext[row_tok].reshape(n_blocks, MOE_BLOCK, d)

    def expert_block(args):
        xe, e = args
        gu = xe @ w_gate_up[e] + b_gate_up[e]
        gate = jnp.minimum(gu[:, :D_FF], SWIGLU_LIMIT)
        up = jnp.clip(gu[:, D_FF:], -SWIGLU_LIMIT, SWIGLU_LIMIT)
        act = (up + 1.0) * gate * jax.nn.sigmoid(SWIGLU_ALPHA * gate)
        return act @ w_down[e] + b_down[e]

    yb = lax.map(expert_block, (xb, blk_e)).reshape(rows, d)
    yb = yb * row_w[:, None].astype(yb.dtype)
    out = jnp.zeros_like(tok_ext).at[row_tok].add(yb)[:n_tok]
    return out.reshape(bsz, n, d)


def setup_inputs(seed: int = 0) -> dict:
    key = jax.random.key(seed)
    ks = jax.random.split(key, 25)
    f32 = jnp.float32
    D = D_MODEL

    def nrm(k, shape, scale):
        return jax.random.normal(k, shape, f32) * scale

    dt0 = jnp.exp(jax.random.uniform(ks[11], (DEPTH, 2, N_HEADS), f32, math.log(1e-3), math.log(1e-1)))
    return {
        'x': nrm(ks[0], (BATCH, SEQ, D), 1.0),
        'c': nrm(ks[1], (BATCH, D), 1.0),
        'ctx': nrm(ks[2], (BATCH, CTX_LEN, D), 1.0),
        'c_ctx': nrm(ks[3], (D,), 1.0),
        'w_mod': nrm(ks[4], (DEPTH, D, N_MOD * D), 0.5 * D ** -0.5),
        'b_mod': nrm(ks[5], (DEPTH, N_MOD * D), 0.02),
        'norm1_w': 1.0 + nrm(ks[6], (DEPTH, D), 0.05),
        'norm2_w': 1.0 + nrm(ks[7], (DEPTH, D), 0.05),
        'w_in': nrm(ks[8], (DEPTH, D, IN_COLS), D ** -0.5),
        'conv_w': nrm(ks[9], (DEPTH, CONV_K, CONV_CH), CONV_K ** -0.5),
        'conv_b': nrm(ks[10], (DEPTH, CONV_CH), 0.02),
        'dt_bias': dt0 + jnp.log(-jnp.expm1(-dt0)),
        'a_log': jnp.log(jax.random.uniform(ks[12], (DEPTH, 2, N_HEADS), f32, 1.0, 16.0)),
        'd_skip': 1.0 + nrm(ks[13], (DEPTH, N_HEADS), 0.1),
        'ssd_norm_w': 1.0 + nrm(ks[14], (DEPTH, D_INNER), 0.05),
        'w_ssd_out': nrm(ks[15], (DEPTH, D_INNER, D), D_INNER ** -0.5),
        'w_four_out': nrm(ks[16], (DEPTH, F_WIDTH, D), F_WIDTH ** -0.5),
        'w_o': nrm(ks[17], (DEPTH, D, D), D ** -0.5),
        'w_router': nrm(ks[18], (DEPTH, D, N_EXPERTS), D ** -0.5),
        'b_router': nrm(ks[19], (DEPTH, N_EXPERTS), 0.01),
        'w_gate_up': nrm(ks[20], (DEPTH, N_EXPERTS, D, 2 * D_FF), D ** -0.5),
        'b_gate_up': nrm(ks[21], (DEPTH, N_EXPERTS, 2 * D_FF), 0.02),
        'w_down': nrm(ks[22], (DEPTH, N_EXPERTS, D_FF, D), D_FF ** -0.5),
        'b_down': nrm(ks[23], (DEPTH, N_EXPERTS, D), 0.02),
        'final_norm_w': 1.0 + nrm(ks[24], (D,), 0.05),
    }


def reference(x, c, ctx, c_ctx, w_mod, b_mod, norm1_w, norm2_w, w_in, conv_w, conv_b, dt_bias,
              a_log, d_skip, ssd_norm_w, w_ssd_out, w_four_out, w_o, w_router, b_router,
              w_gate_up, b_gate_up, w_down, b_down, final_norm_w):
    bsz, n, D = x.shape
    n_ctx = ctx.shape[1]
    h0 = jnp.zeros((bsz, N_GROUPS, HEADS_PER_GROUP, HEAD_DIM, D_STATE), jnp.float32)
    for layer in range(DEPTH):
        mod = jax.nn.silu(c) @ w_mod[layer] + b_mod[layer]
        sh1, sc1, g1, sh2, sc2, g2 = jnp.split(mod[:, None, :], N_MOD, axis=-1)
        mod_c = jax.nn.silu(c_ctx) @ w_mod[layer][:, :2 * D] + b_mod[layer][:2 * D]
        sh1_c, sc1_c = mod_c[:D], mod_c[D:]
        a = -jnp.exp(a_log[layer].astype(jnp.float32))

        hc = rmsnorm(ctx, norm1_w[layer]) * (1.0 + sc1_c) + sh1_c
        pc = hc @ w_in[layer][:, :OFF_CF]
        xb_c = jax.nn.silu(dwconv_centred(pc[..., :XB_CH], conv_w[layer][:, :XB_CH], conv_b[layer][:XB_CH]))
        xc = xb_c[..., :D_INNER].reshape(bsz, n_ctx, N_HEADS, HEAD_DIM)
        bfc = xb_c[..., OFF_BF:OFF_BB].reshape(bsz, n_ctx, N_GROUPS, D_STATE)
        bbc = xb_c[..., OFF_BB:OFF_DT].reshape(bsz, n_ctx, N_GROUPS, D_STATE)
        dtc = jax.nn.softplus(pc[..., OFF_DT:OFF_CF].astype(jnp.float32).reshape(bsz, n_ctx, 2, N_HEADS)
                              + dt_bias[layer].astype(jnp.float32))
        hf_ctx = ssd_final_state(xc, dtc[:, :, 0], a[0], bfc, h0)
        hb_ctx = ssd_final_state(xc[:, ::-1], dtc[:, ::-1, 1], a[1], bbc[:, ::-1], h0)

        h = rmsnorm(x, norm1_w[layer]) * (1.0 + sc1) + sh1
        p = h @ w_in[layer]

        xl = jax.nn.silu(dwconv_centred(p[..., :XB_CH], conv_w[layer][:, :XB_CH], conv_b[layer][:XB_CH]))
        cl = jax.nn.silu(dwconv_centred(p[..., OFF_CF:OFF_Z], conv_w[layer][:, XB_CH:], conv_b[layer][XB_CH:]))
        xs = xl[..., :D_INNER].reshape(bsz, n, N_HEADS, HEAD_DIM)
        bf = xl[..., OFF_BF:OFF_BB].reshape(bsz, n, N_GROUPS, D_STATE)
        bb = xl[..., OFF_BB:OFF_DT].reshape(bsz, n, N_GROUPS, D_STATE)
        cf = cl[..., :BC_WIDTH].reshape(bsz, n, N_GROUPS, D_STATE)
        cb = cl[..., BC_WIDTH:].reshape(bsz, n, N_GROUPS, D_STATE)
        dtl = jax.nn.softplus(p[..., OFF_DT:OFF_CF].astype(jnp.float32).reshape(bsz, n, 2, N_HEADS)
                              + dt_bias[layer].astype(jnp.float32))
        y_f = ssd_scan(xs, dtl[:, :, 0], a[0], bf, cf, hf_ctx)
        y_b = ssd_scan(xs[:, ::-1], dtl[:, ::-1, 1], a[1], bb[:, ::-1], cb[:, ::-1], hb_ctx)[:, ::-1]
        y = y_f + y_b + d_skip[layer].astype(jnp.float32)[:, None] * xs.astype(jnp.float32)
        y = y.reshape(bsz, n, D_INNER).astype(x.dtype)
        z = p[..., OFF_Z:OFF_F]
        y_ssd = rmsnorm(y * jax.nn.silu(z), ssd_norm_w[layer]) @ w_ssd_out[layer]

        u = p[..., OFF_F:OFF_GF].astype(jnp.float32).reshape(bsz, n, F_GROUPS, F_GROUP_DIM)
        u = jnp.fft.fft2(u, axes=(1, 3), norm='ortho').real.reshape(bsz, n, F_WIDTH).astype(x.dtype)
        y_four = u @ w_four_out[layer]

        gate_f = jax.nn.sigmoid(p[..., OFF_GF:OFF_GS])
        gate_s = jax.nn.sigmoid(p[..., OFF_GS:IN_COLS])
        mix = (gate_f * y_four + gate_s * y_ssd) @ w_o[layer]
        x = x + g1 * mix

        h2 = rmsnorm(x, norm2_w[layer]) * (1.0 + sc2) + sh2
        x = x + g2 * moe_ffn(h2, w_router[layer], b_router[layer], w_gate_up[layer], b_gate_up[layer],
                             w_down[layer], b_down[layer])
    return rmsnorm(x, final_norm_w)
```

```python
import numpy as np
import ml_dtypes
import concourse.bass as bass
import concourse.mybir as mybir
from concourse.ap import AP
from concourse.bass_utils import run_bass_kernel_spmd

F32 = mybir.dt.float32
BF16 = mybir.dt.bfloat16
ALU = mybir.AluOpType
AF = mybir.ActivationFunctionType
AX = mybir.AxisListType

D = 1024
SEQ = 4096
NCTX = 256
NT = SEQ // 128
NTC = NCTX // 128
DI = 2048
NH = 32
INC = 9280
OFF_BF, OFF_BB, OFF_DT, OFF_CF, OFF_CB, OFF_Z, OFF_F, OFF_GF, OFF_GS = 2048, 2560, 3072, 3136, 3648, 4160, 6208, 7232, 8256
NE = 32
EPS = 1e-6
EPOCH = 30000
ARENA_BYTES = 212800


class Res:
    __slots__ = ("name", "w", "r")

    def __init__(self, name=""):
        self.name = name
        self.w = None
        self.r = {}


class Kern:
    def __init__(self, nc, ring=12):
        self.nc = nc
        self.eng = {"pe": nc.tensor, "act": nc.scalar, "dve": nc.vector, "pool": nc.gpsimd, "sp": nc.sync}
        self.sems = {}
        self.cnt = {k: 0 for k in self.eng}
        self.seen = {k: {} for k in self.eng}
        self.ring = ring
        self.dsem = {}
        self.dcnt = {k: 0 for k in self.eng}
        self.dseen = {k: set() for k in self.eng}
        self.ninst = 0

    def _csem(self, e, idx):
        ep = (idx - 1) // EPOCH
        key = (e, ep)
        if key not in self.sems:
            self.sems[key] = self.nc.alloc_semaphore(f"c_{e}_{ep}")
        return self.sems[key], idx - ep * EPOCH

    def _dsem(self, q, i):
        key = (q, i % self.ring)
        if key not in self.dsem:
            self.dsem[key] = self.nc.alloc_semaphore(f"d_{q}_{i % self.ring}")
        return self.dsem[key], 16 * (i // self.ring + 1)

    def _wait(self, e, tok):
        if tok is None:
            return
        kind, f, idx = tok
        if kind == "c":
            if f == e and e == "pe":
                return
            if self.seen[e].get(f, 0) >= idx:
                return
            sem, val = self._csem(f, idx)
            self.eng[e].wait_ge(sem, val)
            self.ninst += 1
            self.seen[e][f] = idx
        else:
            if (f, idx) in self.dseen[e]:
                return
            sem, val = self._dsem(f, idx)
            self.eng[e].wait_ge(sem, val)
            self.ninst += 1
            self.dseen[e].add((f, idx))

    def _deps(self, e, reads, writes):
        for r in reads:
            self._wait(e, r.w)
        for w in writes:
            self._wait(e, w.w)
            for tok in list(w.r.values()):
                self._wait(e, tok)

    def _mark(self, tok, reads, writes):
        key = (tok[0], tok[1]) if tok[0] == "c" else tok
        for r in reads:
            r.r[key] = tok
        for w in writes:
            w.w = tok
            w.r = {}

    def op(self, e, fn, reads=(), writes=()):
        self._deps(e, reads, writes)
        ins = fn(self.eng[e])
        self.cnt[e] += 1
        idx = self.cnt[e]
        sem, _ = self._csem(e, idx)
        ins.then_inc(sem, 1)
        self.ninst += 1
        tok = ("c", e, idx)
        self._mark(tok, reads, writes)
        return tok

    def mm(self, fn, reads=(), writes=(), signal=True):
        e = "pe"
        self._deps(e, reads, writes)
        ins = fn(self.eng[e])
        self.ninst += 1
        if signal:
            self.cnt[e] += 1
            idx = self.cnt[e]
            sem, _ = self._csem(e, idx)
            ins.then_inc(sem, 1)
            tok = ("c", e, idx)
        else:
            tok = ("c", e, self.cnt[e] + 1)
        self._mark(tok, reads, writes)
        return tok

    def dma(self, q, out, in_, reads=(), writes=(), **kw):
        i = self.dcnt[q]
        if i >= self.ring:
            self._wait(q, ("d", q, i - self.ring))
        self._deps(q, reads, writes)
        sem, _ = self._dsem(q, i)
        self.eng[q].dma_start(out=out, in_=in_, **kw).then_inc(sem, 16)
        self.ninst += 1
        self.dcnt[q] += 1
        tok = ("d", q, i)
        self._mark(tok, reads, writes)
        return tok

    def idma(self, out, out_off, in_, in_off, bc, reads=(), writes=()):
        q = "pool"
        i = self.dcnt[q]
        if i >= self.ring:
            self._wait(q, ("d", q, i - self.ring))
        self._deps(q, reads, writes)
        sem, _ = self._dsem(q, i)
        self.nc.gpsimd.indirect_dma_start(out=out, out_offset=out_off, in_=in_, in_offset=in_off).then_inc(sem, 16)
        self.ninst += 1
        self.dcnt[q] += 1
        tok = ("d", q, i)
        self._mark(tok, reads, writes)
        return tok

    def guard_begin(self, flag_ap, flag_res, reload=True, thr=0):
        nc = self.nc
        self.gnames = ["pe", "act", "dve", "pool", "sp"]
        if not hasattr(self, "gregs"):
            self.gregs = {e: self.eng[e].alloc_register(f"gflag_{e}") for e in self.gnames}
            self.gset = bass.RegisterHandles([self.gregs[e] for e in self.gnames])
        if reload:
            for e in self.gnames:
                self._wait(e, flag_res.w)
            for e in self.gnames:
                self.eng[e].reg_load(self.gregs[e], flag_ap)
                self.ninst += 1
        self._gcnt0 = dict(self.cnt)
        self._gd0 = dict(self.dcnt)
        self._gseen = {k: dict(v) for k, v in self.seen.items()}
        self._gdseen = {k: set(v) for k, v in self.dseen.items()}
        self._gctx = nc.If_cmp(self.gset, thr, comp_op="IS_GT")
        self._gctx.__enter__()

    def guard_end(self):
        nc = self.nc
        self._gctx.__exit__(None, None, None)
        self.seen = self._gseen
        self.dseen = self._gdseen
        with nc.Else():
            for e in self.gnames:
                eng = self.eng[e]
                if e != "sp":
                    n0, n1 = self._gcnt0[e], self.cnt[e]
                    if n1 > n0:
                        if n0 > 0:
                            sem0, v0 = self._csem(e, n0)
                            eng.wait_ge(sem0, v0)
                        i = n0 + 1
                        while i <= n1:
                            ep = (i - 1) // EPOCH
                            last = min(n1, (ep + 1) * EPOCH)
                            sem, _ = self._csem(e, i)
                            eng.sem_inc(sem, last - i + 1)
                            i = last + 1
                n0, n1 = self._gd0[e], self.dcnt[e]
                for i in range(n0, n1):
                    sem = self.dsem[(e, i % self.ring)]
                    if i >= self.ring:
                        eng.wait_ge(sem, 16 * (i // self.ring))
                    eng.sem_inc(sem, 16)

    def barrier(self):
        for e in self.eng:
            for f in ("pe", "act", "dve", "pool"):
                if self.cnt[f] > 0:
                    self._wait(e, ("c", f, self.cnt[f]))
            for q in self.eng:
                n = self.dcnt[q]
                for i in range(max(0, n - self.ring), n):
                    self._wait(e, ("d", q, i))


class Tile:
    def __init__(self, arena, boff, shape, dt, name):
        self.dt = dt
        esz = 4 if dt == F32 else 2
        self.h = arena.h32 if dt == F32 else arena.h16
        self.ps = arena.n32 if dt == F32 else arena.n32 * 2
        self.base = boff // esz
        self.shape = list(shape)
        dims = [[self.ps, shape[0]]]
        st = int(np.prod(shape[1:]))
        self.fs = st
        for d in shape[1:]:
            st //= d
            dims.append([st, d])
        self.ap = AP(self.h, self.base, dims)
        self.res = Res(name)

    def __getitem__(self, key):
        return self.ap[key]

    def v(self, off, dims, p0=0, np_=None):
        return AP(self.h, p0 * self.ps + self.base + off, [[self.ps, np_ or self.shape[0]]] + [list(d) for d in dims])


class Arena:
    def __init__(self, nc, nbytes):
        self.n32 = nbytes // 4
        self.h32 = nc.alloc_sbuf_tensor("arena", [128, self.n32], F32)
        self.h16 = self.h32.bitcast(BF16)
        self.off = 0
        self.hi = 0
        self.cap = nbytes

    def alloc(self, shape, dt, name=""):
        esz = 4 if dt == F32 else 2
        n = int(np.prod(shape[1:])) * esz
        t = Tile(self, self.off, shape, dt, name)
        self.off += (n + 31) // 32 * 32
        self.hi = max(self.hi, self.off)
        assert self.off <= self.cap, f"SBUF arena overflow at {name}: {self.off}"
        return t

    def mark(self):
        return self.off

    def release(self, m):
        self.off = m


def dram_ap(t, off, dims):
    return AP(t, off, [list(d) for d in dims])


def build(debug=False, stop_after=None, n_exp=NE):
    nc = bass.Bass("TRN2", target_bir_lowering=False)
    K = Kern(nc)
    A = Arena(nc, ARENA_BYTES)
    IN = lambda name, shape, dt=F32: nc.dram_tensor(name, shape, dt, kind="ExternalInput")
    skind = "ExternalOutput" if debug else "Internal"
    SCR = lambda name, shape, dt: nc.dram_tensor(name, shape, dt, kind=skind)

    x_d = IN("x", [SEQ, D]); c_d = IN("c", [1, D]); ctx_d = IN("ctx", [NCTX, D]); cctx_d = IN("c_ctx", [1, D])
    wmod_d = IN("w_mod", [D, 6 * D]); bmod_d = IN("b_mod", [1, 6 * D])
    n1_d = IN("norm1_w", [1, D]); n2_d = IN("norm2_w", [1, D])
    win_d = IN("w_in", [D, INC]); convw_d = IN("conv_w", [5, 4096]); convb_d = IN("conv_b", [1, 4096])
    dtb_d = IN("dt_bias", [1, 64]); alog_d = IN("a_log", [1, 64]); dsk_d = IN("d_skip", [1, NH])
    snw_d = IN("ssd_norm_w", [1, DI]); wso_d = IN("w_ssd_out", [DI, D]); wfo_d = IN("w_four_out", [D, D]); wo_d = IN("w_o", [D, D])
    wr_d = IN("w_router", [D, NE]); br_d = IN("b_router", [1, NE])
    wgu_d = IN("w_gate_up", [NE, D, 2 * D]); bgu_d = IN("b_gate_up", [NE, 2 * D])
    wd_d = IN("w_down", [NE, D, D]); bd_d = IN("b_down", [NE, D]); fn_d = IN("final_norm_w", [1, D])
    ct_d = IN("dft_c", [16, 128, 32 * 256], BF16); nst_d = IN("dft_ns", [16, 128, 32 * 256], BF16)
    cdft_d = IN("cdft", [128, 256], BF16)
    out_d = nc.dram_tensor("out", [SEQ, D], F32, kind="ExternalOutput")

    XS = SCR("XS", [SEQ + NCTX, DI], BF16); BTOK = SCR("BTOK", [SEQ + NCTX, 1024], BF16)
    BT = SCR("BT", [1024, SEQ], BF16); CT = SCR("CT", [1024, SEQ], BF16)
    SZ = SCR("SZ", [SEQ, DI], BF16); G = SCR("G", [SEQ, 2 * D], BF16); V = SCR("V", [SEQ, 8 * 256], BF16)
    YF = SCR("YF", [SEQ, DI], F32); UOT = SCR("UOT", [D, SEQ], BF16); X1 = SCR("X1", [SEQ, D], F32); H2TOK = SCR("H2TOK", [SEQ + 1, 1088], BF16)
    I32 = mybir.dt.int32
    NSLOT = 32768
    WTOK = SCR("WTOK", [SEQ + 1, NE], F32); SLOT_TOK = SCR("SLOT_TOK", [NSLOT, 2], I32); YS = SCR("YS", [NSLOT, D], F32)
    FLAGS = nc.dram_tensor("FLAGS", [1, NE * 8], I32, kind="Internal")
    NBD = nc.dram_tensor("NBD", [1, NE], I32, kind="Internal")
    H2SLOT = nc.dram_tensor("H2SLOT", [NSLOT, 1088], BF16, kind="Internal")
    R_WTOK = Res(); R_SLOT = Res(); R_FLAGS = Res()
    WGU16 = nc.dram_tensor("WGU16", [NE, D, 2 * D], BF16, kind="Internal"); WD16 = nc.dram_tensor("WD16", [NE, D, D], BF16, kind="Internal")

    def precast_expert(e2):
        K.dma("pool", dram_ap(WGU16, e2 * D * 2 * D, [[2 * D, D], [1, 2 * D]]), dram_ap(wgu_d, e2 * D * 2 * D, [[2 * D, D], [1, 2 * D]]), writes=[Res()])
        K.dma("pool", dram_ap(WD16, e2 * D * D, [[D, D], [1, D]]), dram_ap(wd_d, e2 * D * D, [[D, D], [1, D]]), writes=[Res()])
    YFOUR = SCR("YFOUR", [SEQ, D], F32); R_YFOUR = Res()
    R_XS = [Res() for _ in range(NT + NTC)]; R_BTOK = [Res() for _ in range(NT + NTC)]
    R_BT = Res(); R_CT = Res(); R_SZ = Res(); R_G = Res(); R_V = Res(); R_UOT = Res()
    R_YF = [Res() for _ in range(NT)]; R_X1 = [Res() for _ in range(NT)]; R_H2T = Res(); R_OUT = Res()
    dbg = {}

    pb = [nc.alloc_psum_tensor(f"pb{i}", [128, 512], F32) for i in range(8)]
    pbb = [p.bitcast(BF16) for p in pb]
    PB = [Res(f"pb{i}") for i in range(8)]
    bank_ctr = [0]

    def nextbank(lo=0, hi=8):
        b = lo + bank_ctr[0] % (hi - lo)
        bank_ctr[0] += 1
        return b

    ident_f = A.alloc([128, 128], F32, "ident_f"); ident_b = A.alloc([128, 128], BF16, "ident_b")
    ones_f = A.alloc([128, 128], F32, "ones_f")
    triU = A.alloc([128, 128], F32, "triU"); triL = A.alloc([128, 128], F32, "triL")
    striL = A.alloc([128, 128], F32, "striL"); striU = A.alloc([128, 128], F32, "striU")
    epsT = A.alloc([128, 1], F32, "epsT")
    K.op("pool", lambda e: e.memset(ident_f.ap, 0.0), writes=[ident_f.res])
    K.op("pool", lambda e: e.affine_select(out=ident_f.ap, in_=ident_f.ap, compare_op=ALU.not_equal, fill=1.0, base=0,
                                           pattern=[[-1, 128]], channel_multiplier=1), reads=[ident_f.res], writes=[ident_f.res])
    K.op("dve", lambda e: e.tensor_copy(out=ident_b.ap, in_=ident_f.ap), reads=[ident_f.res], writes=[ident_b.res])
    K.op("pool", lambda e: e.memset(ones_f.ap, 1.0), writes=[ones_f.res])
    K.op("dve", lambda e: e.memset(epsT.ap, EPS), writes=[epsT.res])
    for t, cm, pat, cmp_ in ((triU, -1, 1, ALU.is_ge), (triL, 1, -1, ALU.is_ge), (striL, 1, -1, ALU.is_gt), (striU, -1, 1, ALU.is_gt)):
        K.op("pool", lambda e: e.memset(t.ap, 1.0), writes=[t.res])
        K.op("pool", lambda e: e.affine_select(out=t.ap, in_=t.ap, compare_op=cmp_, fill=0.0, base=0, pattern=[[pat, 128]],
                                               channel_multiplier=cm), reads=[t.res], writes=[t.res])

    wS = A.alloc([128, NT, NE], F32, "wS")
    g2b = A.alloc([128, D], F32, "g2b")
    rkS = A.alloc([128, NT, NE], F32, "rkS")
    cnt_b = A.alloc([128, NE], F32, "cnt_b")
    poskS = A.alloc([128, NT, 4], F32, "poskS")
    K.op("dve", lambda e: e.memset(cnt_b.ap, 0.0), writes=[cnt_b.res])
    moe_mark = A.mark()
    modb = A.alloc([128, 6 * D], F32, "modb")
    dtS = A.alloc([128, NT + NTC, 64], F32, "dtS")
    a_b = A.alloc([128, 64], F32, "a_b")
    K.dma("sp", a_b.ap, dram_ap(alog_d, 0, [[0, 128], [1, 64]]), writes=[a_b.res])
    K.op("act", lambda e: e.activation(out=a_b.ap, in_=a_b.ap, func=AF.Exp), reads=[a_b.res], writes=[a_b.res])
    K.op("dve", lambda e: e.tensor_scalar(out=a_b.ap, in0=a_b.ap, scalar1=-1.0, scalar2=None, op0=ALU.mult), reads=[a_b.res], writes=[a_b.res])
    persist_mark = A.mark()

    m0 = A.mark()
    modcb = A.alloc([128, 2 * D], F32, "modcb")
    cS = A.alloc([128, 2, 8], F32, "cS")
    cSb = A.alloc([128, 16, 128], F32, "cSb")
    bmb = A.alloc([128, 6 * D], F32, "bmb")
    n1b = A.alloc([128, D], F32, "n1b"); n2b = A.alloc([128, D], F32, "n2b")
    wm = [A.alloc([128, 8, 512], F32, f"wm{i}") for i in range(2)]
    K.dma("sp", cS.v(0, [[1, 8]]), dram_ap(c_d, 0, [[1, 128], [128, 8]]), writes=[cS.res], allow_slow_non_contiguous=True)
    K.dma("sp", cS.v(8, [[1, 8]]), dram_ap(cctx_d, 0, [[1, 128], [128, 8]]), writes=[cS.res], allow_slow_non_contiguous=True)
    K.dma("sp", bmb.ap, dram_ap(bmod_d, 0, [[0, 128], [1, 6 * D]]), writes=[bmb.res])
    K.dma("sp", n1b.ap, dram_ap(n1_d, 0, [[0, 128], [1, D]]), writes=[n1b.res])
    K.dma("sp", n2b.ap, dram_ap(n2_d, 0, [[0, 128], [1, D]]), writes=[n2b.res])
    K.op("act", lambda e: e.activation(out=cS.ap, in_=cS.ap, func=AF.Silu), reads=[cS.res], writes=[cS.res])
    K.op("dve", lambda e: e.tensor_copy(out=cSb.ap, in_=cS.v(0, [[1, 16], [0, 128]])), reads=[cS.res], writes=[cSb.res])
    for blk in range(12):
        w = wm[blk % 2]
        K.dma("sp", w.ap, dram_ap(wmod_d, blk * 512, [[6 * D, 128], [128 * 6 * D, 8], [1, 512]]), writes=[w.res])
        for j in range(2 if blk < 4 else 1):
            b = nextbank()
            for kc in range(8):
                K.mm(lambda pe: pe.matmul(pb[b][:, :], lhsT=cSb[:, j * 8 + kc, :], rhs=w[:, kc, :], start=(kc == 0), stop=(kc == 7)),
                     reads=[cSb.res, w.res], writes=[PB[b]], signal=(kc == 7))
            dst = modb if j == 0 else modcb
            K.op("dve", lambda e: e.tensor_tensor(out=dst[:, blk * 512:(blk + 1) * 512], in0=pb[b][:, :], in1=bmb[:, blk * 512:(blk + 1) * 512], op=ALU.add),
                 reads=[PB[b], bmb.res], writes=[dst.res])
    K.op("dve", lambda e: e.scalar_tensor_tensor(out=modb[:, D:2 * D], in0=modb[:, D:2 * D], scalar=1.0, in1=n1b.ap, op0=ALU.add, op1=ALU.mult),
         reads=[modb.res, n1b.res], writes=[modb.res])
    K.op("dve", lambda e: e.scalar_tensor_tensor(out=modb[:, 4 * D:5 * D], in0=modb[:, 4 * D:5 * D], scalar=1.0, in1=n2b.ap, op0=ALU.add, op1=ALU.mult),
         reads=[modb.res, n2b.res], writes=[modb.res])
    K.op("dve", lambda e: e.scalar_tensor_tensor(out=modcb[:, D:2 * D], in0=modcb[:, D:2 * D], scalar=1.0, in1=n1b.ap, op0=ALU.add, op1=ALU.mult),
         reads=[modcb.res, n1b.res], writes=[modcb.res])
    K.op("dve", lambda e: e.tensor_copy(out=g2b.ap, in_=modb[:, 5 * D:6 * D]), reads=[modb.res], writes=[g2b.res])
    if debug:
        dbg["modb"] = nc.dram_tensor("dbg_modb", [128, 6 * D], F32, kind="ExternalOutput")
        K.dma("sp", dbg["modb"].ap()[:, :], modb.ap, reads=[modb.res], writes=[Res()])
    K.barrier()
    A.release(m0)
    modcb = A.alloc([128, 2 * D], F32, "modcb")

    HW = SEQ + 4
    HCW = NCTX + 4
    hT = A.alloc([128, 8, HW], BF16, "hT")
    hcT = A.alloc([128, 8, HCW], BF16, "hcT")
    m1 = A.mark()
    xb = [A.alloc([128, D], F32, f"xb{i}") for i in range(2)]
    junk = A.alloc([128, D], BF16, "junk")
    t1 = A.alloc([128, D], F32, "t1")
    hb = [A.alloc([128, D], BF16, f"hb{i}") for i in range(2)]
    ss = A.alloc([128, NT + NTC], F32, "ss"); rs = A.alloc([128, NT + NTC], F32, "rs"); rstd = A.alloc([128, NT + NTC], F32, "rstd")
    for t_, w_ in ((hT, HW), (hcT, HCW)):
        K.op("dve", lambda e: e.memset(t_.v(0, [[w_, 8], [1, 2]]), 0.0), writes=[t_.res])
        K.op("dve", lambda e: e.memset(t_.v(w_ - 2, [[w_, 8], [1, 2]]), 0.0), writes=[t_.res])
    for i in range(NT + NTC):
        xt = xb[i % 2]; hbt = hb[i % 2]
        isctx = i >= NT
        src = dram_ap(ctx_d, (i - NT) * 128 * D, [[D, 128], [1, D]]) if isctx else dram_ap(x_d, i * 128 * D, [[D, 128], [1, D]])
        K.dma("sp", xt.ap, src, writes=[xt.res])
        K.op("act", lambda e: e.activation(out=junk.ap, in_=xt.ap, func=AF.Square, accum_out=ss[:, i:i + 1]), reads=[xt.res], writes=[junk.res, ss.res])
        K.op("act", lambda e: e.activation(out=rs[:, i:i + 1], in_=ss[:, i:i + 1], func=AF.Sqrt, bias=epsT[:, 0:1], scale=1.0 / D),
             reads=[ss.res, epsT.res], writes=[rs.res])
        K.op("dve", lambda e: e.reciprocal(out=rstd[:, i:i + 1], in_=rs[:, i:i + 1]), reads=[rs.res], writes=[rstd.res])
        mb = modcb if isctx else modb
        K.op("dve", lambda e: e.scalar_tensor_tensor(out=t1.ap, in0=xt.ap, scalar=rstd[:, i:i + 1], in1=mb[:, D:2 * D], op0=ALU.mult, op1=ALU.mult),
             reads=[xt.res, rstd.res, mb.res], writes=[t1.res])
        K.op("dve", lambda e: e.tensor_tensor(out=hbt.ap, in0=t1.ap, in1=mb[:, 0:D], op=ALU.add), reads=[t1.res, mb.res], writes=[hbt.res])
        b = nextbank()
        for kc in range(8):
            K.mm(lambda pe: pe.transpose(pbb[b][:, kc * 128:(kc + 1) * 128], hbt[:, kc * 128:(kc + 1) * 128], ident_b.ap),
                 reads=[hbt.res, ident_b.res], writes=[PB[b]], signal=(kc == 7))
        dstT, w_, t0 = (hcT, HCW, (i - NT) * 128) if isctx else (hT, HW, i * 128)
        K.op("act", lambda e: e.copy(out=dstT.v(2 + t0, [[w_, 8], [1, 128]]), in_=AP(pbb[b], 0, [[1024, 128], [128, 8], [1, 128]])),
             reads=[PB[b]], writes=[dstT.res])
    if debug:
        dbg["hT"] = nc.dram_tensor("dbg_hT", [128, 8 * HW], BF16, kind="ExternalOutput")
        K.dma("sp", dbg["hT"].ap()[:, :], hT.v(0, [[1, 8 * HW]]), reads=[hT.res], writes=[Res()])
    K.barrier()
    A.release(m1)
    if stop_after == "p1a":
        return nc, dbg, K, A

    Wb = [A.alloc([128, 8, 512], BF16, f"Wb{i}") for i in range(2)]
    cwT = A.alloc([128, 32, 5], F32, "cwT")
    cbT = A.alloc([128, 32], F32, "cbT")
    dtbb = A.alloc([128, 64], F32, "dtbb")
    cdft = A.alloc([128, 256], BF16, "cdft")
    prows = [A.alloc([128, HW], BF16, f"prow{i}") for i in range(2)]; prow_c = A.alloc([128, HCW], BF16, "prow_c")
    cacc = A.alloc([128, SEQ], F32, "cacc"); cacc_c = A.alloc([128, NCTX], F32, "cacc_c")
    xsT = A.alloc([128, SEQ], BF16, "xsT"); xsT_c = A.alloc([128, NCTX], BF16, "xsT_c")
    tokbuf = A.alloc([128, NT + NTC, 128], BF16, "tokbuf")
    tmpf = [A.alloc([128, 512], F32, f"tmpf{i}") for i in range(2)]
    stg = [A.alloc([128, 512], BF16, f"stg{i}") for i in range(4)]
    uT = [A.alloc([128, 512], BF16, f"uT{i}") for i in range(2)]
    vst = [A.alloc([128, 4, 256], BF16, f"vst{i}") for i in range(2)]
    K.dma("sp", cbT.ap, dram_ap(convb_d, 0, [[1, 128], [128, 32]]), writes=[cbT.res], allow_slow_non_contiguous=True)
    for j in range(5):
        K.dma("sp", cwT.v(j, [[5, 32]]), dram_ap(convw_d, j * 4096, [[1, 128], [128, 32]]), writes=[cwT.res], allow_slow_non_contiguous=True)
    K.dma("sp", dtbb.ap, dram_ap(dtb_d, 0, [[0, 128], [1, 64]]), writes=[dtbb.res])
    K.dma("sp", cdft.ap, cdft_d.ap()[:, :], writes=[cdft.res])
    for t_, w_ in ((prows[0], HW), (prows[1], HW), (prow_c, HCW)):
        K.op("dve", lambda e: e.memset(t_[:, 0:2], 0.0), writes=[t_.res])
        K.op("dve", lambda e: e.memset(t_[:, w_ - 2:w_], 0.0), writes=[t_.res])
    ctr = {"wb": 0, "tmp": 0, "stg": 0, "u": 0, "v": 0, "pc": 0}

    def load_w(col0, ncols=512):
        w = Wb[ctr["wb"] % 2]; ctr["wb"] += 1
        K.dma("pool", w.v(0, [[512, 8], [1, ncols]]), dram_ap(win_d, col0, [[INC, 128], [128 * INC, 8], [1, ncols]]), writes=[w.res])
        return w

    def tile_src(tt):
        return (hcT, (tt - NT) * 128) if tt >= NT else (hT, tt * 128)

    def conv_taps(acc_t, row_t, n, ci):
        K.op("dve", lambda e: e.tensor_scalar(out=acc_t.ap, in0=row_t[:, 0:n], scalar1=cwT[:, ci, 0:1], scalar2=None, op0=ALU.mult),
             reads=[row_t.res, cwT.res], writes=[acc_t.res])
        for j in range(1, 5):
            K.op("dve", lambda e: e.scalar_tensor_tensor(out=acc_t.ap, in0=row_t[:, j:j + n], scalar=cwT[:, ci, j:j + 1], in1=acc_t.ap, op0=ALU.mult, op1=ALU.add),
                 reads=[row_t.res, cwT.res, acc_t.res], writes=[acc_t.res])

    def conv_gen(w, cc, ci, tok_dst, tok_col, tok_width, tok_res, feat_dst, feat_row, feat_res, with_ctx):
        prow = prows[ctr["pc"] % 2]; ctr["pc"] += 1
        for tg in range(8):
            b = nextbank()
            for kc in range(8):
                K.mm(lambda pe: pe.matmul(pb[b][:, :], lhsT=w[:, kc, cc * 128:(cc + 1) * 128], rhs=hT[:, kc, 2 + tg * 512:2 + tg * 512 + 512], start=(kc == 0), stop=(kc == 7)),
                     reads=[hT.res, w.res], writes=[PB[b]], signal=(kc == 7))
            K.op("act", lambda e: e.copy(out=prow[:, 2 + tg * 512:2 + tg * 512 + 512], in_=pb[b][:, :]), reads=[PB[b]], writes=[prow.res])
        yield
        conv_taps(cacc, prow, SEQ, ci)
        K.op("act", lambda e: e.activation(out=xsT.ap, in_=cacc.ap, func=AF.Silu, bias=cbT[:, ci:ci + 1], scale=1.0), reads=[cacc.res, cbT.res], writes=[xsT.res])
        if feat_dst is not None:
            K.dma("sp", dram_ap(feat_dst, feat_row * SEQ, [[SEQ, 128], [1, SEQ]]), xsT.ap, reads=[xsT.res], writes=[feat_res])
        if tok_dst is not None:
            for t8_ in range(4):
                b = nextbank()
                for i8 in range(8):
                    tt = t8_ * 8 + i8
                    K.mm(lambda pe: pe.transpose(pbb[b][:, i8 * 128:(i8 + 1) * 128], xsT[:, tt * 128:(tt + 1) * 128], ident_b.ap),
                         reads=[xsT.res, ident_b.res], writes=[PB[b]], signal=(i8 == 7))
                K.op("act", lambda e: e.copy(out=tokbuf.v(t8_ * 8 * 128, [[1, 1024]]), in_=pbb[b][:, 0:1024]), reads=[PB[b]], writes=[tokbuf.res])
        if with_ctx:
            b = nextbank()
            for kc in range(8):
                K.mm(lambda pe: pe.matmul(pb[b][:, 0:NCTX], lhsT=w[:, kc, cc * 128:(cc + 1) * 128], rhs=hcT[:, kc, 2:2 + NCTX], start=(kc == 0), stop=(kc == 7)),
                     reads=[hcT.res, w.res], writes=[PB[b]], signal=(kc == 7))
            K.op("act", lambda e: e.copy(out=prow_c[:, 2:2 + NCTX], in_=pb[b][:, 0:NCTX]), reads=[PB[b]], writes=[prow_c.res])
            conv_taps(cacc_c, prow_c, NCTX, ci)
            K.op("act", lambda e: e.activation(out=xsT_c.ap, in_=cacc_c.ap, func=AF.Silu, bias=cbT[:, ci:ci + 1], scale=1.0), reads=[cacc_c.res, cbT.res], writes=[xsT_c.res])
            b = nextbank()
            for i8 in range(2):
                K.mm(lambda pe: pe.transpose(pbb[b][:, i8 * 128:(i8 + 1) * 128], xsT_c[:, i8 * 128:(i8 + 1) * 128], ident_b.ap),
                     reads=[xsT_c.res, ident_b.res], writes=[PB[b]], signal=(i8 == 1))
            K.op("act", lambda e: e.copy(out=tokbuf.v(NT * 128, [[1, 256]]), in_=pbb[b][:, 0:256]), reads=[PB[b]], writes=[tokbuf.res])
        if tok_dst is not None:
            for hf in range(2):
                K.dma("sp", dram_ap(tok_dst, hf * 17 * 128 * tok_width + tok_col, [[tok_width, 128], [128 * tok_width, 17], [1, 128]]),
                      tokbuf.v(hf * 17 * 128, [[128, 17], [1, 128]]), reads=[tokbuf.res], writes=[tok_res[hf]])

    def run_pipelined(gens):
        prev = None
        for g_ in gens:
            next(g_)
            if prev is not None:
                for _ in prev:
                    pass
            prev = g_
        if prev is not None:
            for _ in prev:
                pass

    def tok_plain(w, func, dst_d, dcol0, dwidth, dres):
        for tt in range(NT):
            b = nextbank()
            for kc in range(8):
                K.mm(lambda pe: pe.matmul(pb[b][:, :], lhsT=hT[:, kc, 2 + tt * 128:2 + tt * 128 + 128], rhs=w[:, kc, :], start=(kc == 0), stop=(kc == 7)),
                     reads=[hT.res, w.res], writes=[PB[b]], signal=(kc == 7))
            sg = stg[ctr["stg"] % 4]; ctr["stg"] += 1
            K.op("act", lambda e: e.activation(out=sg.ap, in_=pb[b][:, :], func=func), reads=[PB[b]], writes=[sg.res])
            K.dma("sp", dram_ap(dst_d, tt * 128 * dwidth + dcol0, [[dwidth, 128], [1, 512]]), sg.ap, reads=[sg.res], writes=[dres])

    def xb_chunks():
        for blk in range(4):
            w = load_w(blk * 512)
            for cc in range(4):
                yield conv_gen(w, cc, blk * 4 + cc, XS, blk * 512 + cc * 128, DI, R_XS, None, 0, None, True)
        for d_ in range(2):
            w = load_w(OFF_BF + d_ * 512)
            for cc in range(4):
                yield conv_gen(w, cc, 16 + d_ * 4 + cc, BTOK, d_ * 512 + cc * 128, 1024, R_BTOK, BT, d_ * 512 + cc * 128, R_BT, True)
    run_pipelined(xb_chunks())
    w = load_w(OFF_DT, 64)
    for tt in range(NT + NTC):
        src, t0 = tile_src(tt)
        b = nextbank()
        for kc in range(8):
            K.mm(lambda pe: pe.matmul(pb[b][:, 0:64], lhsT=src[:, kc, 2 + t0:2 + t0 + 128], rhs=w[:, kc, 0:64], start=(kc == 0), stop=(kc == 7)),
                 reads=[src.res, w.res], writes=[PB[b]], signal=(kc == 7))
        tf = tmpf[ctr["tmp"] % 2]; ctr["tmp"] += 1
        K.op("dve", lambda e: e.tensor_tensor(out=tf[:, 0:64], in0=pb[b][:, 0:64], in1=dtbb.ap, op=ALU.add), reads=[PB[b], dtbb.res], writes=[tf.res])
        K.op("act", lambda e: e.activation(out=tf[:, 0:64], in_=tf[:, 0:64], func=AF.Exp), reads=[tf.res], writes=[tf.res])
        K.op("act", lambda e: e.activation(out=dtS[:, tt, :], in_=tf[:, 0:64], func=AF.Ln, bias=1.0, scale=1.0), reads=[tf.res], writes=[dtS.res])
    def c_chunks():
        for d_ in range(2):
            w = load_w(OFF_CF + d_ * 512)
            for cc in range(4):
                yield conv_gen(w, cc, 24 + d_ * 4 + cc, None, 0, 0, None, CT, d_ * 512 + cc * 128, R_CT, False)
    run_pipelined(c_chunks())
    for blk in range(4):
        w = load_w(OFF_Z + blk * 512)
        tok_plain(w, AF.Silu, SZ, blk * 512, DI, R_SZ)
    for blk in range(2):
        w = load_w(OFF_F + blk * 512)
        for g in range(4):
            gg = blk * 4 + g
            for tg in range(8):
                b = nextbank()
                for kc in range(8):
                    K.mm(lambda pe: pe.matmul(pb[b][:, :], lhsT=w[:, kc, g * 128:(g + 1) * 128], rhs=hT[:, kc, 2 + tg * 512:2 + tg * 512 + 512],
                                              start=(kc == 0), stop=(kc == 7)), reads=[hT.res, w.res], writes=[PB[b]], signal=(kc == 7))
                u = uT[ctr["u"] % 2]; ctr["u"] += 1
                K.op("act", lambda e: e.copy(out=u.ap, in_=pb[b][:, :]), reads=[PB[b]], writes=[u.res])
                b2 = nextbank()
                b3 = nextbank()
                for sub in range(4):
                    bb_ = b2 if sub < 2 else b3
                    K.mm(lambda pe: pe.matmul(pb[bb_][:, (sub % 2) * 256:(sub % 2) * 256 + 256], lhsT=u[:, sub * 128:(sub + 1) * 128], rhs=cdft.ap, start=True, stop=True),
                         reads=[u.res, cdft.res], writes=[PB[bb_]], signal=(sub % 2 == 1))
                vs_ = vst[ctr["v"] % 2]; ctr["v"] += 1
                K.op("dve", lambda e: e.tensor_copy(out=vs_.v(0, [[1, 512]]), in_=pb[b2][:, :]), reads=[PB[b2]], writes=[vs_.res])
                K.op("dve", lambda e: e.tensor_copy(out=vs_.v(512, [[1, 512]]), in_=pb[b3][:, :]), reads=[PB[b3]], writes=[vs_.res])
                K.dma("sp", dram_ap(V, tg * 512 * 2048 + gg * 256, [[2048, 128], [128 * 2048, 4], [1, 256]]), vs_.ap, reads=[vs_.res], writes=[R_V])
    for blk in range(4):
        w = load_w(OFF_GF + blk * 512)
        tok_plain(w, AF.Sigmoid, G, blk * 512, 2 * D, R_G)
    if debug:
        dbg["dtS"] = nc.dram_tensor("dbg_dtS", [128, (NT + NTC) * 64], F32, kind="ExternalOutput")
        K.dma("sp", dbg["dtS"].ap()[:, :], dtS.v(0, [[1, (NT + NTC) * 64]]), reads=[dtS.res], writes=[Res()])
    K.barrier()
    A.release(persist_mark)
    if stop_after == "p1b":
        return nc, dbg, K, A

    m3 = A.mark()
    Vs = A.alloc([128, 32, 4, 256], BF16, "Vs")
    R_Vs = [Res() for _ in range(32)]
    ctb = [A.alloc([128, 32, 256], BF16, f"ctb{i}") for i in range(2)]
    nsb = [A.alloc([128, 32, 256], BF16, f"nsb{i}") for i in range(2)]
    uos = [A.alloc([128, 256], BF16, f"uos{i}") for i in range(4)]
    fscale = float(1.0 / np.sqrt(4096.0 * 128.0))
    nuo = 0
    for hh in range(2):
        for tc in range(32):
            K.dma("sp", Vs.v(tc * 1024, [[1, 1024]]), dram_ap(V, tc * 128 * 2048 + hh * 1024, [[2048, 128], [1, 1024]]), writes=[R_Vs[tc]])
        for kg in range(16):
            cb_ = ctb[kg % 2]; nb_ = nsb[kg % 2]
            K.dma("sp", cb_.v(0, [[1, 8192]]), dram_ap(ct_d, kg * 128 * 8192, [[8192, 128], [1, 8192]]), writes=[cb_.res])
            K.dma("sp", nb_.v(0, [[1, 8192]]), dram_ap(nst_d, kg * 128 * 8192, [[8192, 128], [1, 8192]]), writes=[nb_.res])
            for g in range(4):
                b = nextbank()
                for tc in range(32):
                    K.mm(lambda pe: pe.matmul(pb[b][:, 0:256], lhsT=Vs[:, tc, g, 0:128], rhs=cb_[:, tc, :], start=(tc == 0), stop=False),
                         reads=[R_Vs[tc], cb_.res], writes=[PB[b]], signal=False)
                    K.mm(lambda pe: pe.matmul(pb[b][:, 0:256], lhsT=Vs[:, tc, g, 128:256], rhs=nb_[:, tc, :], start=False, stop=(tc == 31)),
                         reads=[R_Vs[tc], nb_.res], writes=[PB[b]], signal=(tc == 31))
                u = uos[nuo % 4]; nuo += 1
                K.op("act", lambda e: e.activation(out=u.ap, in_=pb[b][:, 0:256], func=AF.Copy, scale=fscale), reads=[PB[b]], writes=[u.res])
                K.dma("sp", dram_ap(UOT, ((hh * 4 + g) * 128) * SEQ + kg * 256, [[SEQ, 128], [1, 256]]), u.ap, reads=[u.res], writes=[R_UOT])
    K.barrier()
    A.release(m3)
    m3b = A.mark()
    Wfo = A.alloc([128, 8, D], BF16, "Wfo")
    uo_c = [A.alloc([128, 8, 128], BF16, f"uo_c{i}") for i in range(2)]
    yfs = [A.alloc([128, D], F32, f"yfs{i}") for i in range(2)]
    K.dma("pool", Wfo.ap, dram_ap(wfo_d, 0, [[D, 128], [128 * D, 8], [1, D]]), writes=[Wfo.res])
    for c in range(NT):
        uo = uo_c[c % 2]; yf_ = yfs[c % 2]
        K.dma("sp", uo.ap, dram_ap(UOT, c * 128, [[SEQ, 128], [128 * SEQ, 8], [1, 128]]), writes=[uo.res])
        for dh in range(2):
            b = nextbank()
            for k in range(8):
                K.mm(lambda pe: pe.matmul(pb[b][:, :], lhsT=uo[:, k, :], rhs=Wfo[:, k, dh * 512:(dh + 1) * 512], start=(k == 0), stop=(k == 7)),
                     reads=[uo.res, Wfo.res], writes=[PB[b]], signal=(k == 7))
            K.op("act", lambda e: e.copy(out=yf_[:, dh * 512:(dh + 1) * 512], in_=pb[b][:, :]), reads=[PB[b]], writes=[yf_.res])
        K.dma("sp", dram_ap(YFOUR, c * 128 * D, [[D, 128], [1, D]]), yf_.ap, reads=[yf_.res], writes=[R_YFOUR])
    K.barrier()
    A.release(m3b)
    if stop_after == "p3":
        return nc, dbg, K, A

    dsk_b = A.alloc([128, NH], F32, "dsk_b")
    K.dma("sp", dsk_b.ap, dram_ap(dsk_d, 0, [[0, 128], [1, NH]]), writes=[dsk_b.res])
    Hst = [A.alloc([128, DI], F32, f"H{d_}") for d_ in range(2)]
    for d_ in range(2):
        K.op("dve", lambda e: e.memset(Hst[d_].ap, 0.0), writes=[Hst[d_].res])
    m2 = A.mark()

    def ssd_bufs(nb):
        B_ = {}
        B_["nb"] = nb
        B_["xs"] = [A.alloc([128, DI], BF16, "xs_c") for _ in range(nb)]
        B_["btok"] = [A.alloc([128, 512], BF16, "btok_c") for _ in range(nb)]
        B_["bt"] = [A.alloc([128, 4, 128], BF16, "bt_c") for _ in range(nb)]
        B_["ct"] = [A.alloc([128, 4, 128], BF16, "ct_c") for _ in range(nb)]
        B_["dta"] = [A.alloc([128, 32], F32, "dta") for _ in range(2)]
        B_["E"] = [A.alloc([128, 96], F32, "E") for _ in range(2)]
        B_["rseg"] = [A.alloc([128, 512], F32, "rseg") for _ in range(nb)]
        B_["LT"] = [A.alloc([128, 512], F32, "LT") for _ in range(nb)]
        B_["CBm"] = A.alloc([128, 512], F32, "CBm")
        B_["MT"] = [A.alloc([128, 32, 128], BF16, "MT") for _ in range(nb)]
        B_["xdt"] = [A.alloc([128, DI], BF16, "xdt") for _ in range(nb)]
        B_["xdd"] = [A.alloc([128, DI], BF16, "xdd") for _ in range(1)]
        B_["Hb"] = [A.alloc([128, DI], BF16, "Hb") for _ in range(1)]
        B_["yoff"] = [A.alloc([128, 512], F32, "yoff") for _ in range(2)]
        B_["ysb"] = [A.alloc([128, DI], F32, "ysb") for _ in range(nb)]
        B_["i"] = 0
        return B_

    def ssd_step(B_, d_, c, row, want_y):
        i = B_["i"]; B_["i"] += 1
        par = i % B_["nb"]; p2 = i % 2
        incl = triU if d_ == 0 else triL
        excl = striL if d_ == 0 else striU
        valid = incl
        xs_ = B_["xs"][par]; btok_ = B_["btok"][par]; bt_ = B_["bt"][par]; ct_ = B_["ct"][par]
        dta = B_["dta"][p2]; E = B_["E"][p2]; MT = B_["MT"][par]; xdt = B_["xdt"][par]; xdd = B_["xdd"][0]; Hb = B_["Hb"][0]
        ysb = B_["ysb"][par]; CBm = B_["CBm"]; H = Hst[d_]
        K.dma("sp", xs_.ap, dram_ap(XS, row * 128 * DI, [[DI, 128], [1, DI]]), writes=[xs_.res])
        K.dma("sp", btok_.ap, dram_ap(BTOK, row * 128 * 1024 + d_ * 512, [[1024, 128], [1, 512]]), writes=[btok_.res])
        if want_y:
            K.dma("sp", bt_.ap, dram_ap(BT, d_ * 512 * SEQ + c * 128, [[SEQ, 128], [128 * SEQ, 4], [1, 128]]), writes=[bt_.res])
            K.dma("sp", ct_.ap, dram_ap(CT, d_ * 512 * SEQ + c * 128, [[SEQ, 128], [128 * SEQ, 4], [1, 128]]), writes=[ct_.res])
        dtoff = row * 64 + d_ * 32
        K.op("dve", lambda e: e.tensor_tensor(out=dta.ap, in0=dtS.v(dtoff, [[1, 32]]), in1=a_b[:, d_ * 32:(d_ + 1) * 32], op=ALU.mult),
             reads=[dtS.res, a_b.res], writes=[dta.res])
        for n_, lt_ in enumerate((incl, excl, ones_f)):
            K.mm(lambda pe: pe.matmul(pb[0][:, n_ * 32:(n_ + 1) * 32], lhsT=lt_.ap, rhs=dta.ap, start=True, stop=True),
                 reads=[lt_.res, dta.res], writes=[PB[0]], signal=(n_ == 2))
        K.op("act", lambda e: e.activation(out=E.ap, in_=pb[0][:, 0:96], func=AF.Exp), reads=[PB[0]], writes=[E.res])
        if want_y:
            for g in range(4):
                K.mm(lambda pe: pe.matmul(pb[1][:, g * 128:(g + 1) * 128], lhsT=bt_[:, g, :], rhs=ct_[:, g, :], start=True, stop=True),
                     reads=[bt_.res, ct_.res], writes=[PB[1]], signal=(g == 3))
            K.op("dve", lambda e: e.tensor_tensor(out=CBm.v(0, [[128, 4], [1, 128]]), in0=AP(pb[1], 0, [[512, 128], [128, 4], [1, 128]]),
                                                  in1=valid.v(0, [[0, 4], [1, 128]]), op=ALU.mult), reads=[PB[1], valid.res], writes=[CBm.res])
            for q in range(8):
                rs_ = B_["rseg"][q % B_["nb"]]; l_ = B_["LT"][q % B_["nb"]]; bk = 2 + q % 2
                K.op("pool", lambda e: e.tensor_tensor(out=rs_.v(0, [[128, 4], [1, 128]]), in0=incl.v(0, [[0, 4], [1, 128]]),
                                                       in1=dta.v(4 * q, [[1, 4], [0, 128]]), op=ALU.mult), reads=[incl.res, dta.res], writes=[rs_.res])
                K.mm(lambda pe: pe.matmul(pb[bk][:, :], lhsT=excl.ap, rhs=rs_.ap, start=True, stop=True), reads=[excl.res, rs_.res], writes=[PB[bk]])
                K.op("act", lambda e: e.activation(out=l_.ap, in_=pb[bk][:, :], func=AF.Exp), reads=[PB[bk]], writes=[l_.res])
                g = q // 2
                K.op("dve", lambda e: e.tensor_tensor(out=MT.v(4 * q * 128, [[128, 4], [1, 128]]), in0=l_.v(0, [[128, 4], [1, 128]]),
                                                      in1=CBm.v(g * 128, [[0, 4], [1, 128]]), op=ALU.mult), reads=[l_.res, CBm.res], writes=[MT.res])
        K.op("dve", lambda e: e.tensor_tensor(out=xdt.v(0, [[64, 32], [1, 64]]), in0=xs_.v(0, [[64, 32], [1, 64]]),
                                              in1=dtS.v(dtoff, [[1, 32], [0, 64]]), op=ALU.mult), reads=[xs_.res, dtS.res], writes=[xdt.res])
        if want_y:
            K.op("act", lambda e: e.copy(out=Hb.ap, in_=H.ap), reads=[H.res], writes=[Hb.res])
            for g in range(4):
                bd = 4 + g % 2
                yo = B_["yoff"][g % 2]
                for hh in range(8):
                    h = g * 8 + hh
                    K.mm(lambda pe: pe.matmul(pb[bd][:, hh * 64:(hh + 1) * 64], lhsT=MT[:, h, :], rhs=xdt[:, h * 64:(h + 1) * 64], start=True, stop=True),
                         reads=[MT.res, xdt.res], writes=[PB[bd]], signal=(hh == 7))
                K.mm(lambda pe: pe.matmul(pb[6][:, :], lhsT=ct_[:, g, :], rhs=Hb[:, g * 512:(g + 1) * 512], start=True, stop=True),
                     reads=[ct_.res, Hb.res], writes=[PB[6]])
                K.op("dve", lambda e: e.tensor_tensor(out=yo.v(0, [[64, 8], [1, 64]]), in0=AP(pb[6], 0, [[512, 128], [64, 8], [1, 64]]),
                                                      in1=E.v(g * 8, [[1, 8], [0, 64]]), op=ALU.mult), reads=[PB[6], E.res], writes=[yo.res])
                K.op("dve", lambda e: e.tensor_tensor(out=ysb[:, g * 512:(g + 1) * 512], in0=pb[bd][:, :], in1=yo.ap, op=ALU.add),
                     reads=[PB[bd], yo.res], writes=[ysb.res])
        K.op("dve", lambda e: e.tensor_tensor(out=xdd.v(0, [[64, 32], [1, 64]]), in0=xdt.v(0, [[64, 32], [1, 64]]),
                                              in1=E.v(32, [[1, 32], [0, 64]]), op=ALU.mult), reads=[xdt.res, E.res], writes=[xdd.res])
        for g in range(4):
            K.mm(lambda pe: pe.matmul(pb[7][:, :], lhsT=btok_[:, g * 128:(g + 1) * 128], rhs=xdd[:, g * 512:(g + 1) * 512], start=True, stop=True),
                 reads=[btok_.res, xdd.res], writes=[PB[7]])
            K.op("dve", lambda e: e.tensor_tensor(out=H.v(g * 512, [[64, 8], [1, 64]]), in0=H.v(g * 512, [[64, 8], [1, 64]]),
                                                  in1=E.v(64 + g * 8, [[1, 8], [0, 64]]), op=ALU.mult), reads=[H.res, E.res], writes=[H.res])
            K.op("dve", lambda e: e.tensor_tensor(out=H[:, g * 512:(g + 1) * 512], in0=H[:, g * 512:(g + 1) * 512], in1=pb[7][:, :], op=ALU.add),
                 reads=[H.res, PB[7]], writes=[H.res])
        return xs_, ysb

    SB = ssd_bufs(2)
    ssd_step(SB, 0, 0, NT + 0, False)
    ssd_step(SB, 0, 1, NT + 1, False)
    ssd_step(SB, 1, 1, NT + 1, False)
    ssd_step(SB, 1, 0, NT + 0, False)
    if debug:
        dbg["Hctx"] = nc.dram_tensor("dbg_Hctx", [2, 128, DI], F32, kind="ExternalOutput")
        for d_ in range(2):
            K.dma("sp", dram_ap(dbg["Hctx"], d_ * 128 * DI, [[DI, 128], [1, DI]]), Hst[d_].ap, reads=[Hst[d_].res], writes=[Res()])
    for c in range(NT):
        _, ysb = ssd_step(SB, 0, c, c, True)
        K.dma("sp", dram_ap(YF, c * 128 * DI, [[DI, 128], [1, DI]]), ysb.ap, reads=[ysb.res], writes=[R_YF[c]])
    K.barrier()
    A.release(m2)
    if stop_after == "p2a":
        return nc, dbg, K, A

    def subtile(parent, eoff, shape, dt, name):
        t = Tile(A, 0, shape, dt, name)
        esz_p = 4 if parent.dt == F32 else 2
        esz = 4 if dt == F32 else 2
        t.base = (parent.base * esz_p + eoff * esz_p) // esz
        dims = [[t.ps, shape[0]]]
        st = int(np.prod(shape[1:]))
        for d__ in shape[1:]:
            st //= d__
            dims.append([st, d__])
        t.ap = AP(t.h, t.base, dims)
        return t

    x_c = subtile(modb, 0, [128, D], F32, "x_c")
    yfour_c = subtile(modb, D, [128, D], F32, "yfour_c")
    m1 = subtile(modb, 5 * D, [128, D], F32, "m1")
    h2f = m1
    g1b = modb[:, 2 * D:3 * D]; sh2b = modb[:, 3 * D:4 * D]; sc2b = modb[:, 4 * D:5 * D]
    Wso = A.alloc([128, 16, D], BF16, "Wso"); Wo = A.alloc([128, 8, D], BF16, "Wo"); wr = A.alloc([128, 8, NE], F32, "wr")
    R_Wso = [Res() for _ in range(4)]
    snw_b = A.alloc([128, DI], F32, "snw_b"); brb = A.alloc([128, NE], F32, "brb")
    big = A.alloc([128, DI], F32, "big")
    sz_c = A.alloc([128, DI], BF16, "sz_c"); g_c = A.alloc([128, 2 * D], BF16, "g_c")
    gzn = A.alloc([128, DI], BF16, "gzn"); gznT = A.alloc([128, 16, 128], BF16, "gznT")
    merged = A.alloc([128, D], BF16, "merged"); mergedT = A.alloc([128, 8, 128], BF16, "mergedT")
    x1t = A.alloc([128, D], F32, "x1t"); h2T32 = A.alloc([128, 8, 128], F32, "h2T32")
    ssq = A.alloc([128, NT], F32, "ssq"); rq = A.alloc([128, NT], F32, "rq"); rstdq = A.alloc([128, NT], F32, "rstdq")
    ss2 = A.alloc([128, NT], F32, "ss2"); r2 = A.alloc([128, NT], F32, "r2"); rstd2 = A.alloc([128, NT], F32, "rstd2")
    lg = A.alloc([128, NE], F32, "lg"); t8 = A.alloc([128, 8], F32, "t8"); msk = A.alloc([128, NE], F32, "msk")
    negm = A.alloc([128, 1], F32, "negm"); ex = A.alloc([128, NE], F32, "ex"); ssum = A.alloc([128, 1], F32, "ssum"); rsum = A.alloc([128, 1], F32, "rsum")
    for i4 in range(4):
        K.dma("pool", Wso.v(i4 * 4 * D, [[D, 4], [1, D]]), dram_ap(wso_d, i4 * 512 * D, [[D, 128], [128 * D, 4], [1, D]]), writes=[R_Wso[i4]])
    K.dma("pool", Wo.ap, dram_ap(wo_d, 0, [[D, 128], [128 * D, 8], [1, D]]), writes=[Wo.res])
    K.dma("sp", wr.ap, dram_ap(wr_d, 0, [[NE, 128], [128 * NE, 8], [1, NE]]), writes=[wr.res])
    K.dma("sp", snw_b.ap, dram_ap(snw_d, 0, [[0, 128], [1, DI]]), writes=[snw_b.res])
    K.dma("sp", brb.ap, dram_ap(br_d, 0, [[0, 128], [1, NE]]), writes=[brb.res])
    SB1 = ssd_bufs(1)
    rows = lambda t_, c_, w_: dram_ap(t_, c_ * 128 * w_, [[w_, 128], [1, w_]])

    def finish(c, xs_, ysb):
        K.dma("sp", big.ap, rows(YF, c, DI), reads=[R_YF[c]], writes=[big.res])
        K.dma("sp", sz_c.ap, rows(SZ, c, DI), writes=[sz_c.res])
        K.dma("sp", g_c.ap, rows(G, c, 2 * D), writes=[g_c.res])
        K.dma("sp", yfour_c.ap, rows(YFOUR, c, D), writes=[yfour_c.res])
        K.dma("sp", x_c.ap, rows(x_d, c, D), writes=[x_c.res])
        K.op("dve", lambda e: e.tensor_tensor(out=ysb.ap, in0=ysb.ap, in1=big.ap, op=ALU.add), reads=[ysb.res, big.res], writes=[ysb.res])
        K.op("dve", lambda e: e.tensor_tensor(out=big.v(0, [[64, 32], [1, 64]]), in0=xs_.v(0, [[64, 32], [1, 64]]), in1=dsk_b.v(0, [[1, 32], [0, 64]]), op=ALU.mult),
             reads=[xs_.res, dsk_b.res], writes=[big.res])
        K.op("dve", lambda e: e.tensor_tensor(out=ysb.ap, in0=ysb.ap, in1=big.ap, op=ALU.add), reads=[ysb.res, big.res], writes=[ysb.res])
        if debug and c == NT - 1:
            dbg["ylast"] = nc.dram_tensor("dbg_ylast", [128, DI], F32, kind="ExternalOutput")
            K.dma("sp", dbg["ylast"].ap()[:, :], ysb.ap, reads=[ysb.res], writes=[Res()])
        K.op("dve", lambda e: e.tensor_tensor(out=big.ap, in0=ysb.ap, in1=sz_c.ap, op=ALU.mult), reads=[ysb.res, sz_c.res], writes=[big.res])
        K.op("act", lambda e: e.activation(out=gzn.ap, in_=big.ap, func=AF.Square, accum_out=ssq[:, c:c + 1]), reads=[big.res], writes=[gzn.res, ssq.res])
        K.op("act", lambda e: e.activation(out=rq[:, c:c + 1], in_=ssq[:, c:c + 1], func=AF.Sqrt, bias=epsT[:, 0:1], scale=1.0 / DI), reads=[ssq.res, epsT.res], writes=[rq.res])
        K.op("dve", lambda e: e.reciprocal(out=rstdq[:, c:c + 1], in_=rq[:, c:c + 1]), reads=[rq.res], writes=[rstdq.res])
        K.op("dve", lambda e: e.scalar_tensor_tensor(out=gzn.ap, in0=big.ap, scalar=rstdq[:, c:c + 1], in1=snw_b.ap, op0=ALU.mult, op1=ALU.mult),
             reads=[big.res, rstdq.res, snw_b.res], writes=[gzn.res])
        for f in range(16):
            K.mm(lambda pe: pe.transpose(pbb[f // 8][:, (f % 8) * 128:(f % 8 + 1) * 128], gzn[:, f * 128:(f + 1) * 128], ident_b.ap),
                 reads=[gzn.res, ident_b.res], writes=[PB[f // 8]], signal=(f % 8 == 7))
        for hf in range(2):
            K.op("act", lambda e: e.copy(out=gznT.v(hf * 1024, [[1, 1024]]), in_=pbb[hf][:, 0:1024]), reads=[PB[hf]], writes=[gznT.res])
        for dh in range(2):
            for f in range(16):
                K.mm(lambda pe: pe.matmul(pb[2 + dh][:, :], lhsT=gznT[:, f, :], rhs=Wso[:, f, dh * 512:(dh + 1) * 512], start=(f == 0), stop=(f == 15)),
                     reads=[gznT.res, R_Wso[f // 4]], writes=[PB[2 + dh]], signal=(f == 15))
            K.op("dve", lambda e: e.tensor_tensor(out=m1[:, dh * 512:(dh + 1) * 512], in0=pb[2 + dh][:, :], in1=g_c[:, D + dh * 512:D + (dh + 1) * 512], op=ALU.mult),
                 reads=[PB[2 + dh], g_c.res], writes=[m1.res])
        K.op("dve", lambda e: e.tensor_tensor(out=yfour_c.ap, in0=yfour_c.ap, in1=g_c[:, 0:D], op=ALU.mult), reads=[yfour_c.res, g_c.res], writes=[yfour_c.res])
        K.op("dve", lambda e: e.tensor_tensor(out=merged.ap, in0=m1.ap, in1=yfour_c.ap, op=ALU.add), reads=[m1.res, yfour_c.res], writes=[merged.res])
        for k in range(8):
            K.mm(lambda pe: pe.transpose(pbb[6][:, k * 128:(k + 1) * 128], merged[:, k * 128:(k + 1) * 128], ident_b.ap),
                 reads=[merged.res, ident_b.res], writes=[PB[6]], signal=(k == 7))
        K.op("act", lambda e: e.copy(out=mergedT.v(0, [[1, 1024]]), in_=pbb[6][:, 0:1024]), reads=[PB[6]], writes=[mergedT.res])
        for dh in range(2):
            for k in range(8):
                K.mm(lambda pe: pe.matmul(pb[4 + dh][:, :], lhsT=mergedT[:, k, :], rhs=Wo[:, k, dh * 512:(dh + 1) * 512], start=(k == 0), stop=(k == 7)),
                     reads=[mergedT.res, Wo.res], writes=[PB[4 + dh]], signal=(k == 7))
            K.op("dve", lambda e: e.tensor_tensor(out=x1t[:, dh * 512:(dh + 1) * 512], in0=pb[4 + dh][:, :], in1=g1b[:, dh * 512:(dh + 1) * 512], op=ALU.mult),
                 reads=[PB[4 + dh], modb.res], writes=[x1t.res])
        K.op("dve", lambda e: e.tensor_tensor(out=x1t.ap, in0=x1t.ap, in1=x_c.ap, op=ALU.add), reads=[x1t.res, x_c.res], writes=[x1t.res])
        K.dma("sp", rows(X1, c, D), x1t.ap, reads=[x1t.res], writes=[R_X1[c]])
        K.op("act", lambda e: e.activation(out=gzn[:, 0:D], in_=x1t.ap, func=AF.Square, accum_out=ss2[:, c:c + 1]), reads=[x1t.res], writes=[gzn.res, ss2.res])
        K.op("act", lambda e: e.activation(out=r2[:, c:c + 1], in_=ss2[:, c:c + 1], func=AF.Sqrt, bias=epsT[:, 0:1], scale=1.0 / D), reads=[ss2.res, epsT.res], writes=[r2.res])
        K.op("dve", lambda e: e.reciprocal(out=rstd2[:, c:c + 1], in_=r2[:, c:c + 1]), reads=[r2.res], writes=[rstd2.res])
        K.op("dve", lambda e: e.scalar_tensor_tensor(out=h2f.ap, in0=x1t.ap, scalar=rstd2[:, c:c + 1], in1=sc2b, op0=ALU.mult, op1=ALU.mult),
             reads=[x1t.res, rstd2.res, modb.res], writes=[h2f.res])
        K.op("dve", lambda e: e.tensor_tensor(out=h2f.ap, in0=h2f.ap, in1=sh2b, op=ALU.add), reads=[h2f.res, modb.res], writes=[h2f.res])
        for k in range(8):
            K.mm(lambda pe: pe.transpose(pb[k // 4][:, (k % 4) * 128:(k % 4 + 1) * 128], h2f[:, k * 128:(k + 1) * 128], ident_f.ap),
                 reads=[h2f.res, ident_f.res], writes=[PB[k // 4]], signal=(k % 4 == 3))
        for hf in range(2):
            K.op("act", lambda e: e.copy(out=h2T32.v(hf * 512, [[1, 512]]), in_=pb[hf][:, :]), reads=[PB[hf]], writes=[h2T32.res])
        K.op("act", lambda e: e.copy(out=merged.ap, in_=h2f.ap), reads=[h2f.res], writes=[merged.res])
        K.dma("sp", dram_ap(H2TOK, c * 128 * 1088, [[1088, 128], [1, D]]), merged.ap, reads=[merged.res], writes=[R_H2T])
        for k in range(8):
            K.mm(lambda pe: pe.matmul(pb[7][:, 0:NE], lhsT=h2T32[:, k, :], rhs=wr[:, k, :], start=(k == 0), stop=(k == 7)),
                 reads=[h2T32.res, wr.res], writes=[PB[7]], signal=(k == 7))
        K.op("dve", lambda e: e.tensor_tensor(out=lg.ap, in0=pb[7][:, 0:NE], in1=brb.ap, op=ALU.add), reads=[PB[7], brb.res], writes=[lg.res])
        K.op("dve", lambda e: e.max(out=t8.ap, in_=lg.ap), reads=[lg.res], writes=[t8.res])
        K.op("dve", lambda e: e.tensor_scalar(out=msk.ap, in0=lg.ap, scalar1=t8[:, 3:4], scalar2=None, op0=ALU.is_ge), reads=[lg.res, t8.res], writes=[msk.res])
        K.mm(lambda pe: pe.matmul(pb[7][:, 32:64], lhsT=triU.ap, rhs=msk.ap, start=True, stop=True), reads=[triU.res, msk.res], writes=[PB[7]], signal=False)
        K.mm(lambda pe: pe.matmul(pb[7][:, 64:96], lhsT=ones_f.ap, rhs=msk.ap, start=True, stop=True), reads=[ones_f.res, msk.res], writes=[PB[7]])
        K.op("dve", lambda e: e.tensor_tensor(out=rkS[:, c, :], in0=pb[7][:, 32:64], in1=msk.ap, op=ALU.subtract), reads=[PB[7], msk.res], writes=[rkS.res])
        K.op("dve", lambda e: e.tensor_tensor(out=rkS[:, c, :], in0=rkS[:, c, :], in1=cnt_b.ap, op=ALU.add), reads=[rkS.res, cnt_b.res], writes=[rkS.res])
        K.op("dve", lambda e: e.tensor_tensor(out=cnt_b.ap, in0=cnt_b.ap, in1=pb[7][:, 64:96], op=ALU.add), reads=[cnt_b.res, PB[7]], writes=[cnt_b.res])
        K.op("dve", lambda e: e.tensor_scalar(out=negm.ap, in0=t8[:, 0:1], scalar1=-1.0, scalar2=None, op0=ALU.mult), reads=[t8.res], writes=[negm.res])
        K.op("act", lambda e: e.activation(out=ex.ap, in_=lg.ap, func=AF.Exp, bias=negm[:, 0:1], scale=1.0), reads=[lg.res, negm.res], writes=[ex.res])
        K.op("dve", lambda e: e.tensor_tensor(out=ex.ap, in0=ex.ap, in1=msk.ap, op=ALU.mult), reads=[ex.res, msk.res], writes=[ex.res])
        K.op("dve", lambda e: e.reduce_sum(out=ssum.ap, in_=ex.ap, axis=AX.X), reads=[ex.res], writes=[ssum.res])
        K.op("dve", lambda e: e.reciprocal(out=rsum.ap, in_=ssum.ap), reads=[ssum.res], writes=[rsum.res])
        K.op("dve", lambda e: e.tensor_scalar(out=wS[:, c, :], in0=ex.ap, scalar1=rsum[:, 0:1], scalar2=None, op0=ALU.mult), reads=[ex.res, rsum.res], writes=[wS.res])
        K.dma("sp", dram_ap(H2TOK, c * 128 * 1088 + D, [[1088, 128], [1, 64]]), wS[:, c, :].bitcast(BF16), reads=[wS.res], writes=[R_WTOK])

    for c in range(NT - 1, -1, -1):
        xs_, ysb = ssd_step(SB1, 1, c, c, True)
        finish(c, xs_, ysb)
    if debug:
        dbg["wS"] = nc.dram_tensor("dbg_wS", [128, NT * NE], F32, kind="ExternalOutput")
        K.dma("sp", dbg["wS"].ap()[:, :], wS.v(0, [[1, NT * NE]]), reads=[wS.res], writes=[Res()])
    K.barrier()
    A.release(moe_mark)
    if stop_after == "p2b":
        return nc, dbg, K, A

    IO = bass.IndirectOffsetOnAxis
    iv = lambda ap: ap.bitcast(I32)
    pstart_b = A.alloc([128, NE], F32, "pstart_b")
    iota4 = A.alloc([128, 4], F32, "iota4")
    m5 = A.mark()
    flT = A.alloc([128, NE, 8], F32, "flT"); fli = A.alloc([128, NE * 8], F32, "fli")
    nb_b = A.alloc([128, NE], F32, "nb_b"); d1 = A.alloc([128, NE], F32, "d1"); t32 = A.alloc([128, NE], F32, "t32"); t8b = A.alloc([128, 8], F32, "t8b"); p4 = A.alloc([128, 4], F32, "p4")
    io4i = A.alloc([128, 4], F32, "io4i")
    for j in range(8):
        K.op("dve", lambda e: e.tensor_scalar(out=flT.v(j, [[8, NE]]), in0=cnt_b.ap, scalar1=512.0 * j, scalar2=None, op0=ALU.is_gt), reads=[cnt_b.res], writes=[flT.res])
    K.op("dve", lambda e: e.reduce_sum(out=nb_b.ap, in_=flT.ap, axis=AX.X), reads=[flT.res], writes=[nb_b.res])
    K.op("dve", lambda e: e.memset(pstart_b[:, 0:1], 0.0), writes=[pstart_b.res])
    for ei in range(1, NE):
        K.op("dve", lambda e: e.tensor_tensor(out=pstart_b[:, ei:ei + 1], in0=pstart_b[:, ei - 1:ei], in1=nb_b[:, ei - 1:ei], op=ALU.add),
             reads=[pstart_b.res, nb_b.res], writes=[pstart_b.res])
    K.op("dve", lambda e: e.tensor_scalar(out=pstart_b.ap, in0=pstart_b.ap, scalar1=512.0, scalar2=None, op0=ALU.mult), reads=[pstart_b.res], writes=[pstart_b.res])
    K.op("dve", lambda e: e.tensor_copy(out=iv(fli.ap)[:, 0:NE], in_=nb_b.ap), reads=[nb_b.res, fli.res], writes=[fli.res])
    K.dma("sp", NBD.ap()[:, :], iv(fli.ap)[0:1, 0:NE], reads=[fli.res], writes=[R_FLAGS])
    K.op("pool", lambda e: e.iota(iv(io4i.ap), pattern=[[128, 4]], base=0, channel_multiplier=1), writes=[io4i.res])
    K.op("dve", lambda e: e.tensor_copy(out=iota4.ap, in_=iv(io4i.ap)), reads=[io4i.res], writes=[iota4.res])
    zt = A.alloc([128, 8 * 1088], BF16, "zt")
    h2w = [A.alloc([128, 1088], BF16, f"h2w{i}") for i in range(2)]
    K.op("pool", lambda e: e.memset(zt.ap, 0.0), writes=[zt.res])
    for i32 in range(32):
        K.dma("sp", dram_ap(H2SLOT, i32 * 1024 * 1088, [[8 * 1088, 128], [1, 8 * 1088]]), zt.ap, reads=[zt.res], writes=[Res()])
    K.barrier()
    for c in range(NT):
        K.op("dve", lambda e: e.scalar_tensor_tensor(out=d1.ap, in0=rkS[:, c, :], scalar=1.0, in1=pstart_b.ap, op0=ALU.add, op1=ALU.add),
             reads=[rkS.res, pstart_b.res], writes=[d1.res])
        K.op("dve", lambda e: e.tensor_scalar(out=t32.ap, in0=wS[:, c, :], scalar1=0.0, scalar2=None, op0=ALU.is_gt), reads=[wS.res], writes=[t32.res])
        K.op("dve", lambda e: e.tensor_tensor(out=d1.ap, in0=d1.ap, in1=t32.ap, op=ALU.mult), reads=[d1.res, t32.res], writes=[d1.res])
        K.op("dve", lambda e: e.max(out=t8b.ap, in_=d1.ap), reads=[d1.res], writes=[t8b.res])
        K.op("dve", lambda e: e.tensor_scalar(out=p4.ap, in0=t8b[:, 0:4], scalar1=-1.0, scalar2=0.0, op0=ALU.add, op1=ALU.max), reads=[t8b.res], writes=[p4.res])
        K.op("dve", lambda e: e.tensor_copy(out=iv(poskS[:, c, :]), in_=p4.ap), reads=[p4.res], writes=[poskS.res])
        hw_ = h2w[c % 2]
        K.dma("sp", hw_.ap, dram_ap(H2TOK, c * 128 * 1088, [[1088, 128], [1, 1088]]), writes=[hw_.res])
        for k in range(4):
            K.idma(out=H2SLOT.ap()[:, :], out_off=IO(ap=iv(poskS[:, c, k:k + 1]), axis=0), in_=hw_.ap, in_off=None, bc=NSLOT - 1,
                   reads=[poskS.res, hw_.res], writes=[Res()])
    if debug:
        dbg["cnt"] = nc.dram_tensor("dbg_cnt", [128, NE], F32, kind="ExternalOutput")
        K.dma("sp", dbg["cnt"].ap()[:, :], cnt_b.ap, reads=[cnt_b.res], writes=[Res()])
    K.barrier()
    A.release(m5)
    if stop_after == "p5a":
        return nc, dbg, K, A

    Wgu = [A.alloc([128, 8, 2 * D], BF16, f"Wgu{i}") for i in range(2)]
    Wd = [A.alloc([128, 8, D], BF16, f"Wd{i}") for i in range(2)]
    R_Wgu = [[Res() for _ in range(4)] for _ in range(2)]
    R_Wd = [[Res() for _ in range(2)] for _ in range(2)]
    bguT = A.alloc([128, 16, NE], F32, "bguT")
    m5b = A.mark()
    hg = A.alloc([128, 4, 1088], BF16, "hg"); R_hg = [Res() for _ in range(4)]
    h2cT = A.alloc([128, 8, 512], BF16, "h2cT")
    actT = [A.alloc([128, 8, 512], BF16, f"actT{i}") for i in range(2)]
    ys = A.alloc([128, 4, D], F32, "ys"); R_ys = [Res() for _ in range(4)]
    gtb = [A.alloc([128, 512], F32, f"gt{i}") for i in range(2)]; stb = [A.alloc([128, 512], F32, f"st{i}") for i in range(2)]
    utb = [A.alloc([128, 512], F32, f"ut{i}") for i in range(2)]
    nel = [0]
    posf = A.alloc([128, 4], F32, "posf"); posi = A.alloc([128, 4], F32, "posi")
    bgs = subtile(hg, 0, [NE, 2 * D], F32, "bgs")
    K.dma("sp", bgs.ap, bgu_d.ap()[:, :], writes=[bgs.res])
    for cch in range(16):
        b = nextbank()
        K.mm(lambda pe: pe.transpose(pb[b][:, 0:NE], bgs[:, cch * 128:(cch + 1) * 128], ident_f[0:NE, 0:NE]), reads=[bgs.res, ident_f.res], writes=[PB[b]])
        K.op("dve", lambda e: e.tensor_copy(out=bguT[:, cch, :], in_=pb[b][:, 0:NE]), reads=[PB[b]], writes=[bguT.res])
    bguT1 = A.alloc([128, 8, NE], F32, "bguT1")
    K.op("dve", lambda e: e.tensor_scalar(out=bguT1.ap, in0=bguT[:, 8:16, :], scalar1=1.0, scalar2=None, op0=ALU.add), reads=[bguT.res], writes=[bguT1.res])
    K.barrier()
    nact = 0
    ncp = 0
    wstage = [A.alloc([128, 8, 512], F32, f"wstage{i}") for i in range(2)]
    nstg = [0]

    def load_piece(e2, pc):
        stt = wstage[nstg[0] % 2]; nstg[0] += 1
        if pc < 4:
            src = dram_ap(wgu_d, e2 * D * 2 * D + pc * 512, [[2 * D, 128], [128 * 2 * D, 8], [1, 512]])
            dst = Wgu[e2 % 2].v(pc * 512, [[2 * D, 8], [1, 512]]); res_ = R_Wgu[e2 % 2][pc]
        else:
            src = dram_ap(wd_d, e2 * D * D + (pc - 4) * 512, [[D, 128], [128 * D, 8], [1, 512]])
            dst = Wd[e2 % 2].v((pc - 4) * 512, [[D, 8], [1, 512]]); res_ = R_Wd[e2 % 2][pc - 4]
        K.dma("sp", stt.ap, src, writes=[stt.res])
        K.op("act", lambda e: e.copy(out=dst, in_=stt.ap), reads=[stt.res], writes=[res_])

    for pc in range(6):
        load_piece(0, pc)
    for ei in range(n_exp):
        wg = Wgu[ei % 2]; wdn = Wd[ei % 2]; rg = R_Wgu[ei % 2]; rd = R_Wd[ei % 2]
        for j in range(8):
            K.guard_begin(NBD.ap()[0:1, ei:ei + 1], R_FLAGS, reload=(j == 0), thr=j)
            K.op("dve", lambda e: e.tensor_scalar(out=posf.ap, in0=iota4.ap, scalar1=pstart_b[:, ei:ei + 1], scalar2=float(j * 512), op0=ALU.add, op1=ALU.add),
                 reads=[iota4.res, pstart_b.res], writes=[posf.res])
            K.op("dve", lambda e: e.tensor_copy(out=iv(posi.ap), in_=posf.ap), reads=[posf.res], writes=[posi.res])
            for sub in range(4):
                K.idma(out=hg[:, sub, :], out_off=None, in_=H2SLOT.ap()[:, :], in_off=IO(ap=iv(posi[:, sub:sub + 1]), axis=0), bc=NSLOT - 1, reads=[posi.res], writes=[R_hg[sub]])
            for kk in range(4):
                b = nextbank(0, 4)
                for k2 in range(2):
                    k = 2 * kk + k2
                    for sub in range(4):
                        K.mm(lambda pe: pe.transpose(pbb[b][:, k2 * 512 + sub * 128:k2 * 512 + (sub + 1) * 128], hg[:, sub, k * 128:(k + 1) * 128], ident_b.ap),
                             reads=[R_hg[sub], ident_b.res], writes=[PB[b]], signal=(k2 == 1 and sub == 3))
                if ncp % 2 == 0:
                    K.op("act", lambda e: e.copy(out=h2cT.v(2 * kk * 512, [[1, 1024]]), in_=pbb[b][:, 0:1024]), reads=[PB[b]], writes=[h2cT.res])
                else:
                    K.op("dve", lambda e: e.tensor_copy(out=h2cT.v(2 * kk * 512, [[1, 1024]]), in_=pbb[b][:, 0:1024]), reads=[PB[b]], writes=[h2cT.res])
                ncp += 1
            at = actT[nact % 2]; nact += 1
            for jj in range(8):
                bg_ = nextbank(0, 4); bu_ = nextbank(0, 4)
                for k in range(8):
                    K.mm(lambda pe: pe.matmul(pb[bg_][:, :], lhsT=wg[:, k, jj * 128:(jj + 1) * 128], rhs=h2cT[:, k, :], start=(k == 0), stop=(k == 7)),
                         reads=[rg[jj // 4], h2cT.res], writes=[PB[bg_]], signal=(k == 7))
                for k in range(8):
                    K.mm(lambda pe: pe.matmul(pb[bu_][:, :], lhsT=wg[:, k, D + jj * 128:D + (jj + 1) * 128], rhs=h2cT[:, k, :], start=(k == 0), stop=(k == 7)),
                         reads=[rg[2 + jj // 4], h2cT.res], writes=[PB[bu_]], signal=(k == 7))
                gt = gtb[nel[0] % 2]; st_ = stb[nel[0] % 2]; ut = utb[nel[0] % 2]; nel[0] += 1
                K.op("dve", lambda e: e.tensor_scalar(out=gt.ap, in0=pb[bg_][:, :], scalar1=bguT[:, jj, ei:ei + 1], scalar2=7.0, op0=ALU.add, op1=ALU.min),
                     reads=[PB[bg_], bguT.res], writes=[gt.res])
                K.op("act", lambda e: e.activation(out=st_.ap, in_=gt.ap, func=AF.Silu, scale=1.702), reads=[gt.res], writes=[st_.res])
                K.op("dve", lambda e: e.tensor_scalar(out=ut.ap, in0=pb[bu_][:, :], scalar1=bguT1[:, jj, ei:ei + 1], scalar2=-6.0, op0=ALU.add, op1=ALU.max),
                     reads=[PB[bu_], bguT1.res], writes=[ut.res])
                K.op("dve", lambda e: e.scalar_tensor_tensor(out=at[:, jj, :], in0=ut.ap, scalar=8.0, in1=st_.ap, op0=ALU.min, op1=ALU.mult), reads=[ut.res, st_.res], writes=[at.res])
            for sub in range(4):
                for dh in range(2):
                    bo = nextbank(4, 8)
                    for f in range(8):
                        K.mm(lambda pe: pe.matmul(pb[bo][:, :], lhsT=at[:, f, sub * 128:(sub + 1) * 128], rhs=wdn[:, f, dh * 512:(dh + 1) * 512], start=(f == 0), stop=(f == 7)),
                             reads=[at.res, rd[dh]], writes=[PB[bo]], signal=(f == 7))
                    K.op("dve", lambda e: e.tensor_scalar(out=ys[:, sub, dh * 512:(dh + 1) * 512], in0=pb[bo][:, :], scalar1=hg[:, sub, D:D + 64].bitcast(F32)[:, ei:ei + 1], scalar2=float(1.0 / 1.702), op0=ALU.mult, op1=ALU.mult),
                         reads=[PB[bo], R_hg[sub]], writes=[R_ys[sub]])
                K.idma(out=YS.ap()[:, :], out_off=IO(ap=iv(posi[:, sub:sub + 1]), axis=0), in_=ys[:, sub, :], in_off=None, bc=NSLOT - 1,
                       reads=[R_ys[sub], posi.res], writes=[Res()])
            K.guard_end()
            if ei + 1 < n_exp and j < 2:
                for pc in range(3 * j, 3 * j + 3):
                    load_piece(ei + 1, pc)
    K.barrier()
    A.release(m5b)

    yk2 = [A.alloc([128, 4, D], F32, f"yk{i}") for i in range(2)]; R_yk2 = [[Res() for _ in range(4)] for _ in range(2)]
    accb2 = [A.alloc([128, D], F32, f"accb{i}") for i in range(2)]
    bdn = A.alloc([NE, D], F32, "bdn")
    wT2 = [A.alloc([NE, 128], F32, f"wT{i}") for i in range(2)]
    fnb = A.alloc([128, D], F32, "fnb")
    xo2 = [A.alloc([128, D], F32, f"xo{i}") for i in range(2)]
    ss3 = A.alloc([128, NT], F32, "ss3"); r3 = A.alloc([128, NT], F32, "r3"); rstd3 = A.alloc([128, NT], F32, "rstd3")
    K.dma("sp", fnb.ap, dram_ap(fn_d, 0, [[0, 128], [1, D]]), writes=[fnb.res])
    K.dma("sp", bdn.ap, bd_d.ap()[:, :], writes=[bdn.res])
    for c in range(NT):
        yk = yk2[c % 2]; R_yk = R_yk2[c % 2]; accb = accb2[c % 2]; wT = wT2[c % 2]; xo = xo2[c % 2]
        b = nextbank()
        K.mm(lambda pe: pe.transpose(pb[b][0:NE, 0:128], wS[:, c, :], ident_f.ap), reads=[wS.res, ident_f.res], writes=[PB[b]])
        K.op("dve", lambda e: e.tensor_copy(out=wT.ap, in_=pb[b][0:NE, 0:128]), reads=[PB[b]], writes=[wT.res])
        K.dma("sp", xo.ap, rows(X1, c, D), writes=[xo.res])
        for k in range(4):
            K.idma(out=yk[:, k, :], out_off=None, in_=YS.ap()[:, :], in_off=IO(ap=iv(poskS[:, c, k:k + 1]), axis=0), bc=NSLOT - 1, reads=[poskS.res], writes=[R_yk[k]])
        for dh in range(2):
            b2 = nextbank()
            K.mm(lambda pe: pe.matmul(pb[b2][:, :], lhsT=wT.ap, rhs=bdn[:, dh * 512:(dh + 1) * 512], start=True, stop=True), reads=[wT.res, bdn.res], writes=[PB[b2]])
            K.op("dve", lambda e: e.tensor_tensor(out=accb[:, dh * 512:(dh + 1) * 512], in0=pb[b2][:, :], in1=yk[:, 0, dh * 512:(dh + 1) * 512], op=ALU.add),
                 reads=[PB[b2], R_yk[0]], writes=[accb.res])
        for k in range(1, 4):
            K.op("dve", lambda e: e.tensor_tensor(out=accb.ap, in0=accb.ap, in1=yk[:, k, :], op=ALU.add), reads=[accb.res, R_yk[k]], writes=[accb.res])
        K.op("dve", lambda e: e.tensor_tensor(out=accb.ap, in0=accb.ap, in1=g2b.ap, op=ALU.mult), reads=[accb.res, g2b.res], writes=[accb.res])
        K.op("dve", lambda e: e.tensor_tensor(out=xo.ap, in0=xo.ap, in1=accb.ap, op=ALU.add), reads=[xo.res, accb.res], writes=[xo.res])
        K.op("act", lambda e: e.activation(out=accb.ap, in_=xo.ap, func=AF.Square, accum_out=ss3[:, c:c + 1]), reads=[xo.res], writes=[accb.res, ss3.res])
        K.op("act", lambda e: e.activation(out=r3[:, c:c + 1], in_=ss3[:, c:c + 1], func=AF.Sqrt, bias=epsT[:, 0:1], scale=1.0 / D),
             reads=[ss3.res, epsT.res], writes=[r3.res])
        K.op("dve", lambda e: e.reciprocal(out=rstd3[:, c:c + 1], in_=r3[:, c:c + 1]), reads=[r3.res], writes=[rstd3.res])
        K.op("dve", lambda e: e.scalar_tensor_tensor(out=xo.ap, in0=xo.ap, scalar=rstd3[:, c:c + 1], in1=fnb.ap, op0=ALU.mult, op1=ALU.mult),
             reads=[xo.res, rstd3.res, fnb.res], writes=[xo.res])
        K.dma("sp", rows(out_d, c, D), xo.ap, reads=[xo.res], writes=[R_OUT])
    K.barrier()
    return nc, dbg, K, A


def host_consts():
    t = np.arange(4096, dtype=np.int64)
    m = (t[:, None] * t[None, :]) % 4096
    ang = 2.0 * np.pi * m.astype(np.float64) / 4096.0
    c = np.cos(ang).astype(np.float32); ns = (-np.sin(ang)).astype(np.float32)

    def lay(a):
        a = a.reshape(32, 128, 16, 256).transpose(2, 1, 0, 3).reshape(16, 128, 32 * 256)
        return np.ascontiguousarray(a).astype(ml_dtypes.bfloat16)
    cc = np.arange(128, dtype=np.int64)
    a2 = 2.0 * np.pi * ((cc[:, None] * cc[None, :]) % 128).astype(np.float64) / 128.0
    cd = np.concatenate([np.cos(a2), np.sin(a2)], axis=1).astype(np.float32).astype(ml_dtypes.bfloat16)
    return lay(c), lay(ns), cd


_CONSTS = None


def make_in_maps(inputs, cores):
    global _CONSTS
    if _CONSTS is None:
        _CONSTS = host_consts()
    ct, nst, cd = _CONSTS
    f = lambda a: np.ascontiguousarray(np.asarray(a, dtype=np.float32))
    shared = {
        "c_ctx": f(inputs["c_ctx"]).reshape(1, D), "w_mod": f(inputs["w_mod"][0]), "b_mod": f(inputs["b_mod"]).reshape(1, 6 * D),
        "norm1_w": f(inputs["norm1_w"]).reshape(1, D), "norm2_w": f(inputs["norm2_w"]).reshape(1, D),
        "w_in": f(inputs["w_in"][0]), "conv_w": f(inputs["conv_w"][0]), "conv_b": f(inputs["conv_b"]).reshape(1, 4096),
        "dt_bias": f(inputs["dt_bias"]).reshape(1, 64), "a_log": f(inputs["a_log"]).reshape(1, 64), "d_skip": f(inputs["d_skip"]).reshape(1, NH),
        "ssd_norm_w": f(inputs["ssd_norm_w"]).reshape(1, DI), "w_ssd_out": f(inputs["w_ssd_out"][0]), "w_four_out": f(inputs["w_four_out"][0]),
        "w_o": f(inputs["w_o"][0]), "w_router": f(inputs["w_router"][0]), "b_router": f(inputs["b_router"]).reshape(1, NE),
        "w_gate_up": f(inputs["w_gate_up"][0]), "b_gate_up": f(inputs["b_gate_up"][0]), "w_down": f(inputs["w_down"][0]),
        "b_down": f(inputs["b_down"][0]), "final_norm_w": f(inputs["final_norm_w"]).reshape(1, D),
        "dft_c": ct, "dft_ns": nst, "cdft": cd,
    }
    maps = []
    for b in cores:
        m = dict(shared)
        m["x"] = f(inputs["x"][b]); m["c"] = f(inputs["c"][b]).reshape(1, D); m["ctx"] = f(inputs["ctx"][b])
        maps.append(m)
    return maps


def kernel(**inputs):
    nc, _, _, _ = build()
    maps = make_in_maps(inputs, list(range(8)))
    res = run_bass_kernel_spmd(nc, maps, core_ids=list(range(8)))
    return np.stack([np.asarray(r["out"], dtype=np.float32) for r in res.results], axis=0)
```

```python
import numpy as np
import ml_dtypes
import concourse.bass as bass
import concourse.mybir as mybir
from concourse.ap import AP
from concourse.bass_utils import run_bass_kernel_spmd

F32 = mybir.dt.float32
BF16 = mybir.dt.bfloat16
ALU = mybir.AluOpType
AF = mybir.ActivationFunctionType
AX = mybir.AxisListType

D = 1024
SEQ = 4096
NCTX = 256
NT = SEQ // 128
NTC = NCTX // 128
DI = 2048
NH = 32
INC = 9280
OFF_BF, OFF_BB, OFF_DT, OFF_CF, OFF_CB, OFF_Z, OFF_F, OFF_GF, OFF_GS = 2048, 2560, 3072, 3136, 3648, 4160, 6208, 7232, 8256
NE = 32
EPS = 1e-6
EPOCH = 30000
ARENA_BYTES = 212800


class Res:
    __slots__ = ("name", "w", "r")

    def __init__(self, name=""):
        self.name = name
        self.w = None
        self.r = {}


class Kern:
    def __init__(self, nc, ring=12):
        self.nc = nc
        self.eng = {"pe": nc.tensor, "act": nc.scalar, "dve": nc.vector, "pool": nc.gpsimd, "sp": nc.sync}
        self.sems = {}
        self.cnt = {k: 0 for k in self.eng}
        self.seen = {k: {} for k in self.eng}
        self.ring = ring
        self.dsem = {}
        self.dcnt = {k: 0 for k in self.eng}
        self.dseen = {k: set() for k in self.eng}
        self.ninst = 0

    def _csem(self, e, idx):
        ep = (idx - 1) // EPOCH
        key = (e, ep)
        if key not in self.sems:
            self.sems[key] = self.nc.alloc_semaphore(f"c_{e}_{ep}")
        return self.sems[key], idx - ep * EPOCH

    def _dsem(self, q, i):
        key = (q, i % self.ring)
        if key not in self.dsem:
            self.dsem[key] = self.nc.alloc_semaphore(f"d_{q}_{i % self.ring}")
        return self.dsem[key], 16 * (i // self.ring + 1)

    def _wait(self, e, tok):
        if tok is None:
            return
        kind, f, idx = tok
        if kind == "c":
            if f == e and e == "pe":
                return
            if self.seen[e].get(f, 0) >= idx:
                return
            sem, val = self._csem(f, idx)
            self.eng[e].wait_ge(sem, val)
            self.ninst += 1
            self.seen[e][f] = idx
        else:
            if (f, idx) in self.dseen[e]:
                return
            sem, val = self._dsem(f, idx)
            self.eng[e].wait_ge(sem, val)
            self.ninst += 1
            self.dseen[e].add((f, idx))

    def _deps(self, e, reads, writes):
        for r in reads:
            self._wait(e, r.w)
        for w in writes:
            self._wait(e, w.w)
            for tok in list(w.r.values()):
                self._wait(e, tok)

    def _mark(self, tok, reads, writes):
        key = (tok[0], tok[1]) if tok[0] == "c" else tok
        for r in reads:
            r.r[key] = tok
        for w in writes:
            w.w = tok
            w.r = {}

    def op(self, e, fn, reads=(), writes=()):
        self._deps(e, reads, writes)
        ins = fn(self.eng[e])
        self.cnt[e] += 1
        idx = self.cnt[e]
        sem, _ = self._csem(e, idx)
        ins.then_inc(sem, 1)
        self.ninst += 1
        tok = ("c", e, idx)
        self._mark(tok, reads, writes)
        return tok

    def mm(self, fn, reads=(), writes=(), signal=True):
        e = "pe"
        self._deps(e, reads, writes)
        ins = fn(self.eng[e])
        self.ninst += 1
        if signal:
            self.cnt[e] += 1
            idx = self.cnt[e]
            sem, _ = self._csem(e, idx)
            ins.then_inc(sem, 1)
            tok = ("c", e, idx)
        else:
            tok = ("c", e, self.cnt[e] + 1)
        self._mark(tok, reads, writes)
        return tok

    def dma(self, q, out, in_, reads=(), writes=(), **kw):
        i = self.dcnt[q]
        if i >= self.ring:
            self._wait(q, ("d", q, i - self.ring))
        self._deps(q, reads, writes)
        sem, _ = self._dsem(q, i)
        self.eng[q].dma_start(out=out, in_=in_, **kw).then_inc(sem, 16)
        self.ninst += 1
        self.dcnt[q] += 1
        tok = ("d", q, i)
        self._mark(tok, reads, writes)
        return tok

    def idma(self, out, out_off, in_, in_off, bc, reads=(), writes=()):
        q = "pool"
        i = self.dcnt[q]
        if i >= self.ring:
            self._wait(q, ("d", q, i - self.ring))
        self._deps(q, reads, writes)
        sem, _ = self._dsem(q, i)
        self.nc.gpsimd.indirect_dma_start(out=out, out_offset=out_off, in_=in_, in_offset=in_off).then_inc(sem, 16)
        self.ninst += 1
        self.dcnt[q] += 1
        tok = ("d", q, i)
        self._mark(tok, reads, writes)
        return tok

    def guard_begin(self, flag_ap, flag_res, reload=True, thr=0):
        nc = self.nc
        self.gnames = ["pe", "act", "dve", "pool", "sp"]
        if not hasattr(self, "gregs"):
            self.gregs = {e: self.eng[e].alloc_register(f"gflag_{e}") for e in self.gnames}
            self.gset = bass.RegisterHandles([self.gregs[e] for e in self.gnames])
        if reload:
            for e in self.gnames:
                self._wait(e, flag_res.w)
            for e in self.gnames:
                self.eng[e].reg_load(self.gregs[e], flag_ap)
                self.ninst += 1
        self._gcnt0 = dict(self.cnt)
        self._gd0 = dict(self.dcnt)
        self._gseen = {k: dict(v) for k, v in self.seen.items()}
        self._gdseen = {k: set(v) for k, v in self.dseen.items()}
        self._gctx = nc.If_cmp(self.gset, thr, comp_op="IS_GT")
        self._gctx.__enter__()

    def guard_end(self):
        nc = self.nc
        self._gctx.__exit__(None, None, None)
        self.seen = self._gseen
        self.dseen = self._gdseen
        with nc.Else():
            for e in self.gnames:
                eng = self.eng[e]
                if e != "sp":
                    n0, n1 = self._gcnt0[e], self.cnt[e]
                    if n1 > n0:
                        if n0 > 0:
                            sem0, v0 = self._csem(e, n0)
                            eng.wait_ge(sem0, v0)
                        i = n0 + 1
                        while i <= n1:
                            ep = (i - 1) // EPOCH
                            last = min(n1, (ep + 1) * EPOCH)
                            sem, _ = self._csem(e, i)
                            eng.sem_inc(sem, last - i + 1)
                            i = last + 1
                n0, n1 = self._gd0[e], self.dcnt[e]
                for i in range(n0, n1):
                    sem = self.dsem[(e, i % self.ring)]
                    if i >= self.ring:
                        eng.wait_ge(sem, 16 * (i // self.ring))
                    eng.sem_inc(sem, 16)

    def barrier(self):
        for e in self.eng:
            for f in ("pe", "act", "dve", "pool"):
                if self.cnt[f] > 0:
                    self._wait(e, ("c", f, self.cnt[f]))
            for q in self.eng:
                n = self.dcnt[q]
                for i in range(max(0, n - self.ring), n):
                    self._wait(e, ("d", q, i))


class Tile:
    def __init__(self, arena, boff, shape, dt, name):
        self.dt = dt
        esz = 4 if dt == F32 else 2
        self.h = arena.h32 if dt == F32 else arena.h16
        self.ps = arena.n32 if dt == F32 else arena.n32 * 2
        self.base = boff // esz
        self.shape = list(shape)
        dims = [[self.ps, shape[0]]]
        st = int(np.prod(shape[1:]))
        self.fs = st
        for d in shape[1:]:
            st //= d
            dims.append([st, d])
        self.ap = AP(self.h, self.base, dims)
        self.res = Res(name)

    def __getitem__(self, key):
        return self.ap[key]

    def v(self, off, dims, p0=0, np_=None):
        return AP(self.h, p0 * self.ps + self.base + off, [[self.ps, np_ or self.shape[0]]] + [list(d) for d in dims])


class Arena:
    def __init__(self, nc, nbytes):
        self.n32 = nbytes // 4
        self.h32 = nc.alloc_sbuf_tensor("arena", [128, self.n32], F32)
        self.h16 = self.h32.bitcast(BF16)
        self.off = 0
        self.hi = 0
        self.cap = nbytes

    def alloc(self, shape, dt, name=""):
        esz = 4 if dt == F32 else 2
        n = int(np.prod(shape[1:])) * esz
        t = Tile(self, self.off, shape, dt, name)
        self.off += (n + 31) // 32 * 32
        self.hi = max(self.hi, self.off)
        assert self.off <= self.cap, f"SBUF arena overflow at {name}: {self.off}"
        return t

    def mark(self):
        return self.off

    def release(self, m):
        self.off = m


def dram_ap(t, off, dims):
    return AP(t, off, [list(d) for d in dims])


def build(debug=False, stop_after=None, n_exp=NE):
    nc = bass.Bass("TRN2", target_bir_lowering=False)
    K = Kern(nc)
    A = Arena(nc, ARENA_BYTES)
    IN = lambda name, shape, dt=F32: nc.dram_tensor(name, shape, dt, kind="ExternalInput")
    skind = "ExternalOutput" if debug else "Internal"
    SCR = lambda name, shape, dt: nc.dram_tensor(name, shape, dt, kind=skind)

    x_d = IN("x", [SEQ, D]); c_d = IN("c", [1, D]); ctx_d = IN("ctx", [NCTX, D]); cctx_d = IN("c_ctx", [1, D])
    wmod_d = IN("w_mod", [D, 6 * D]); bmod_d = IN("b_mod", [1, 6 * D])
    n1_d = IN("norm1_w", [1, D]); n2_d = IN("norm2_w", [1, D])
    win_d = IN("w_in", [D, INC]); convw_d = IN("conv_w", [5, 4096]); convb_d = IN("conv_b", [1, 4096])
    dtb_d = IN("dt_bias", [1, 64]); alog_d = IN("a_log", [1, 64]); dsk_d = IN("d_skip", [1, NH])
    snw_d = IN("ssd_norm_w", [1, DI]); wso_d = IN("w_ssd_out", [DI, D]); wfo_d = IN("w_four_out", [D, D]); wo_d = IN("w_o", [D, D])
    wr_d = IN("w_router", [D, NE]); br_d = IN("b_router", [1, NE])
    wgu_d = IN("w_gate_up", [NE, D, 2 * D]); bgu_d = IN("b_gate_up", [NE, 2 * D])
    wd_d = IN("w_down", [NE, D, D]); bd_d = IN("b_down", [NE, D]); fn_d = IN("final_norm_w", [1, D])
    ct_d = IN("dft_c", [16, 128, 32 * 256], BF16); nst_d = IN("dft_ns", [16, 128, 32 * 256], BF16)
    cdft_d = IN("cdft", [128, 256], BF16)
    out_d = nc.dram_tensor("out", [SEQ, D], F32, kind="ExternalOutput")

    XS = SCR("XS", [SEQ + NCTX, DI], BF16); BTOK = SCR("BTOK", [SEQ + NCTX, 1024], BF16)
    BT = SCR("BT", [1024, SEQ], BF16); CT = SCR("CT", [1024, SEQ], BF16)
    SZ = SCR("SZ", [SEQ, DI], BF16); G = SCR("G", [SEQ, 2 * D], BF16); V = SCR("V", [SEQ, 8 * 256], BF16)
    YF = SCR("YF", [SEQ, DI], F32); UOT = SCR("UOT", [D, SEQ], BF16); X1 = SCR("X1", [SEQ, D], F32); H2TOK = SCR("H2TOK", [SEQ + 1, 1088], BF16)
    I32 = mybir.dt.int32
    NSLOT = 32768
    WTOK = SCR("WTOK", [SEQ + 1, NE], F32); SLOT_TOK = SCR("SLOT_TOK", [NSLOT, 2], I32); YS = SCR("YS", [NSLOT, D], F32)
    FLAGS = nc.dram_tensor("FLAGS", [1, NE * 8], I32, kind="Internal")
    NBD = nc.dram_tensor("NBD", [1, NE], I32, kind="Internal")
    H2SLOT = nc.dram_tensor("H2SLOT", [NSLOT, 1088], BF16, kind="Internal")
    R_WTOK = Res(); R_SLOT = Res(); R_FLAGS = Res()
    WGU16 = nc.dram_tensor("WGU16", [NE, D, 2 * D], BF16, kind="Internal"); WD16 = nc.dram_tensor("WD16", [NE, D, D], BF16, kind="Internal")

    def precast_expert(e2):
        K.dma("pool", dram_ap(WGU16, e2 * D * 2 * D, [[2 * D, D], [1, 2 * D]]), dram_ap(wgu_d, e2 * D * 2 * D, [[2 * D, D], [1, 2 * D]]), writes=[Res()])
        K.dma("pool", dram_ap(WD16, e2 * D * D, [[D, D], [1, D]]), dram_ap(wd_d, e2 * D * D, [[D, D], [1, D]]), writes=[Res()])
    YFOUR = SCR("YFOUR", [SEQ, D], F32); R_YFOUR = Res()
    R_XS = [Res() for _ in range(NT + NTC)]; R_BTOK = [Res() for _ in range(NT + NTC)]
    R_BT = Res(); R_CT = Res(); R_SZ = Res(); R_G = Res(); R_V = Res(); R_UOT = Res()
    R_YF = [Res() for _ in range(NT)]; R_X1 = [Res() for _ in range(NT)]; R_H2T = Res(); R_OUT = Res()
    dbg = {}

    pb = [nc.alloc_psum_tensor(f"pb{i}", [128, 512], F32) for i in range(8)]
    pbb = [p.bitcast(BF16) for p in pb]
    PB = [Res(f"pb{i}") for i in range(8)]
    bank_ctr = [0]

    def nextbank(lo=0, hi=8):
        b = lo + bank_ctr[0] % (hi - lo)
        bank_ctr[0] += 1
        return b

    ident_f = A.alloc([128, 128], F32, "ident_f"); ident_b = A.alloc([128, 128], BF16, "ident_b")
    ones_f = A.alloc([128, 128], F32, "ones_f")
    triU = A.alloc([128, 128], F32, "triU"); triL = A.alloc([128, 128], F32, "triL")
    striL = A.alloc([128, 128], F32, "striL"); striU = A.alloc([128, 128], F32, "striU")
    epsT = A.alloc([128, 1], F32, "epsT")
    K.op("pool", lambda e: e.memset(ident_f.ap, 0.0), writes=[ident_f.res])
    K.op("pool", lambda e: e.affine_select(out=ident_f.ap, in_=ident_f.ap, compare_op=ALU.not_equal, fill=1.0, base=0,
                                           pattern=[[-1, 128]], channel_multiplier=1), reads=[ident_f.res], writes=[ident_f.res])
    K.op("dve", lambda e: e.tensor_copy(out=ident_b.ap, in_=ident_f.ap), reads=[ident_f.res], writes=[ident_b.res])
    K.op("pool", lambda e: e.memset(ones_f.ap, 1.0), writes=[ones_f.res])
    K.op("dve", lambda e: e.memset(epsT.ap, EPS), writes=[epsT.res])
    for t, cm, pat, cmp_ in ((triU, -1, 1, ALU.is_ge), (triL, 1, -1, ALU.is_ge), (striL, 1, -1, ALU.is_gt), (striU, -1, 1, ALU.is_gt)):
        K.op("pool", lambda e: e.memset(t.ap, 1.0), writes=[t.res])
        K.op("pool", lambda e: e.affine_select(out=t.ap, in_=t.ap, compare_op=cmp_, fill=0.0, base=0, pattern=[[pat, 128]],
                                               channel_multiplier=cm), reads=[t.res], writes=[t.res])

    wS = A.alloc([128, NT, NE], F32, "wS")
    g2b = A.alloc([128, D], F32, "g2b")
    rkS = A.alloc([128, NT, NE], F32, "rkS")
    cnt_b = A.alloc([128, NE], F32, "cnt_b")
    poskS = A.alloc([128, NT, 4], F32, "poskS")
    K.op("dve", lambda e: e.memset(cnt_b.ap, 0.0), writes=[cnt_b.res])
    moe_mark = A.mark()
    modb = A.alloc([128, 6 * D], F32, "modb")
    dtS = A.alloc([128, NT + NTC, 64], F32, "dtS")
    a_b = A.alloc([128, 64], F32, "a_b")
    K.dma("sp", a_b.ap, dram_ap(alog_d, 0, [[0, 128], [1, 64]]), writes=[a_b.res])
    K.op("act", lambda e: e.activation(out=a_b.ap, in_=a_b.ap, func=AF.Exp), reads=[a_b.res], writes=[a_b.res])
    K.op("dve", lambda e: e.tensor_scalar(out=a_b.ap, in0=a_b.ap, scalar1=-1.0, scalar2=None, op0=ALU.mult), reads=[a_b.res], writes=[a_b.res])
    persist_mark = A.mark()

    m0 = A.mark()
    modcb = A.alloc([128, 2 * D], F32, "modcb")
    cS = A.alloc([128, 2, 8], F32, "cS")
    cSb = A.alloc([128, 16, 128], F32, "cSb")
    bmb = A.alloc([128, 6 * D], F32, "bmb")
    n1b = A.alloc([128, D], F32, "n1b"); n2b = A.alloc([128, D], F32, "n2b")
    wm = [A.alloc([128, 8, 512], F32, f"wm{i}") for i in range(2)]
    K.dma("sp", cS.v(0, [[1, 8]]), dram_ap(c_d, 0, [[1, 128], [128, 8]]), writes=[cS.res], allow_slow_non_contiguous=True)
    K.dma("sp", cS.v(8, [[1, 8]]), dram_ap(cctx_d, 0, [[1, 128], [128, 8]]), writes=[cS.res], allow_slow_non_contiguous=True)
    K.dma("sp", bmb.ap, dram_ap(bmod_d, 0, [[0, 128], [1, 6 * D]]), writes=[bmb.res])
    K.dma("sp", n1b.ap, dram_ap(n1_d, 0, [[0, 128], [1, D]]), writes=[n1b.res])
    K.dma("sp", n2b.ap, dram_ap(n2_d, 0, [[0, 128], [1, D]]), writes=[n2b.res])
    K.op("act", lambda e: e.activation(out=cS.ap, in_=cS.ap, func=AF.Silu), reads=[cS.res], writes=[cS.res])
    K.op("dve", lambda e: e.tensor_copy(out=cSb.ap, in_=cS.v(0, [[1, 16], [0, 128]])), reads=[cS.res], writes=[cSb.res])
    for blk in range(12):
        w = wm[blk % 2]
        K.dma("sp", w.ap, dram_ap(wmod_d, blk * 512, [[6 * D, 128], [128 * 6 * D, 8], [1, 512]]), writes=[w.res])
        for j in range(2 if blk < 4 else 1):
            b = nextbank()
            for kc in range(8):
                K.mm(lambda pe: pe.matmul(pb[b][:, :], lhsT=cSb[:, j * 8 + kc, :], rhs=w[:, kc, :], start=(kc == 0), stop=(kc == 7)),
                     reads=[cSb.res, w.res], writes=[PB[b]], signal=(kc == 7))
            dst = modb if j == 0 else modcb
            K.op("dve", lambda e: e.tensor_tensor(out=dst[:, blk * 512:(blk + 1) * 512], in0=pb[b][:, :], in1=bmb[:, blk * 512:(blk + 1) * 512], op=ALU.add),
                 reads=[PB[b], bmb.res], writes=[dst.res])
    K.op("dve", lambda e: e.scalar_tensor_tensor(out=modb[:, D:2 * D], in0=modb[:, D:2 * D], scalar=1.0, in1=n1b.ap, op0=ALU.add, op1=ALU.mult),
         reads=[modb.res, n1b.res], writes=[modb.res])
    K.op("dve", lambda e: e.scalar_tensor_tensor(out=modb[:, 4 * D:5 * D], in0=modb[:, 4 * D:5 * D], scalar=1.0, in1=n2b.ap, op0=ALU.add, op1=ALU.mult),
         reads=[modb.res, n2b.res], writes=[modb.res])
    K.op("dve", lambda e: e.scalar_tensor_tensor(out=modcb[:, D:2 * D], in0=modcb[:, D:2 * D], scalar=1.0, in1=n1b.ap, op0=ALU.add, op1=ALU.mult),
         reads=[modcb.res, n1b.res], writes=[modcb.res])
    K.op("dve", lambda e: e.tensor_copy(out=g2b.ap, in_=modb[:, 5 * D:6 * D]), reads=[modb.res], writes=[g2b.res])
    if debug:
        dbg["modb"] = nc.dram_tensor("dbg_modb", [128, 6 * D], F32, kind="ExternalOutput")
        K.dma("sp", dbg["modb"].ap()[:, :], modb.ap, reads=[modb.res], writes=[Res()])
    K.barrier()
    A.release(m0)
    modcb = A.alloc([128, 2 * D], F32, "modcb")

    HW = SEQ + 4
    HCW = NCTX + 4
    hT = A.alloc([128, 8, HW], BF16, "hT")
    hcT = A.alloc([128, 8, HCW], BF16, "hcT")
    m1 = A.mark()
    xb = [A.alloc([128, D], F32, f"xb{i}") for i in range(2)]
    junk = A.alloc([128, D], BF16, "junk")
    t1 = A.alloc([128, D], F32, "t1")
    hb = [A.alloc([128, D], BF16, f"hb{i}") for i in range(2)]
    ss = A.alloc([128, NT + NTC], F32, "ss"); rs = A.alloc([128, NT + NTC], F32, "rs"); rstd = A.alloc([128, NT + NTC], F32, "rstd")
    for t_, w_ in ((hT, HW), (hcT, HCW)):
        K.op("dve", lambda e: e.memset(t_.v(0, [[w_, 8], [1, 2]]), 0.0), writes=[t_.res])
        K.op("dve", lambda e: e.memset(t_.v(w_ - 2, [[w_, 8], [1, 2]]), 0.0), writes=[t_.res])
    for i in range(NT + NTC):
        xt = xb[i % 2]; hbt = hb[i % 2]
        isctx = i >= NT
        src = dram_ap(ctx_d, (i - NT) * 128 * D, [[D, 128], [1, D]]) if isctx else dram_ap(x_d, i * 128 * D, [[D, 128], [1, D]])
        K.dma("sp", xt.ap, src, writes=[xt.res])
        K.op("act", lambda e: e.activation(out=junk.ap, in_=xt.ap, func=AF.Square, accum_out=ss[:, i:i + 1]), reads=[xt.res], writes=[junk.res, ss.res])
        K.op("act", lambda e: e.activation(out=rs[:, i:i + 1], in_=ss[:, i:i + 1], func=AF.Sqrt, bias=epsT[:, 0:1], scale=1.0 / D),
             reads=[ss.res, epsT.res], writes=[rs.res])
        K.op("dve", lambda e: e.reciprocal(out=rstd[:, i:i + 1], in_=rs[:, i:i + 1]), reads=[rs.res], writes=[rstd.res])
        mb = modcb if isctx else modb
        K.op("dve", lambda e: e.scalar_tensor_tensor(out=t1.ap, in0=xt.ap, scalar=rstd[:, i:i + 1], in1=mb[:, D:2 * D], op0=ALU.mult, op1=ALU.mult),
             reads=[xt.res, rstd.res, mb.res], writes=[t1.res])
        K.op("dve", lambda e: e.tensor_tensor(out=hbt.ap, in0=t1.ap, in1=mb[:, 0:D], op=ALU.add), reads=[t1.res, mb.res], writes=[hbt.res])
        b = nextbank()
        for kc in range(8):
            K.mm(lambda pe: pe.transpose(pbb[b][:, kc * 128:(kc + 1) * 128], hbt[:, kc * 128:(kc + 1) * 128], ident_b.ap),
                 reads=[hbt.res, ident_b.res], writes=[PB[b]], signal=(kc == 7))
        dstT, w_, t0 = (hcT, HCW, (i - NT) * 128) if isctx else (hT, HW, i * 128)
        K.op("act", lambda e: e.copy(out=dstT.v(2 + t0, [[w_, 8], [1, 128]]), in_=AP(pbb[b], 0, [[1024, 128], [128, 8], [1, 128]])),
             reads=[PB[b]], writes=[dstT.res])
    if debug:
        dbg["hT"] = nc.dram_tensor("dbg_hT", [128, 8 * HW], BF16, kind="ExternalOutput")
        K.dma("sp", dbg["hT"].ap()[:, :], hT.v(0, [[1, 8 * HW]]), reads=[hT.res], writes=[Res()])
    K.barrier()
    A.release(m1)
    if stop_after == "p1a":
        return nc, dbg, K, A

    Wb = [A.alloc([128, 8, 512], BF16, f"Wb{i}") for i in range(2)]
    cwT = A.alloc([128, 32, 5], F32, "cwT")
    cbT = A.alloc([128, 32], F32, "cbT")
    dtbb = A.alloc([128, 64], F32, "dtbb")
    cdft = A.alloc([128, 256], BF16, "cdft")
    prows = [A.alloc([128, HW], BF16, f"prow{i}") for i in range(2)]; prow_c = A.alloc([128, HCW], BF16, "prow_c")
    cacc = A.alloc([128, SEQ], F32, "cacc"); cacc_c = A.alloc([128, NCTX], F32, "cacc_c")
    xsT = A.alloc([128, SEQ], BF16, "xsT"); xsT_c = A.alloc([128, NCTX], BF16, "xsT_c")
    tokbuf = A.alloc([128, NT + NTC, 128], BF16, "tokbuf")
    tmpf = [A.alloc([128, 512], F32, f"tmpf{i}") for i in range(2)]
    stg = [A.alloc([128, 512], BF16, f"stg{i}") for i in range(4)]
    uT = [A.alloc([128, 512], BF16, f"uT{i}") for i in range(2)]
    vst = [A.alloc([128, 4, 256], BF16, f"vst{i}") for i in range(2)]
    K.dma("sp", cbT.ap, dram_ap(convb_d, 0, [[1, 128], [128, 32]]), writes=[cbT.res], allow_slow_non_contiguous=True)
    for j in range(5):
        K.dma("sp", cwT.v(j, [[5, 32]]), dram_ap(convw_d, j * 4096, [[1, 128], [128, 32]]), writes=[cwT.res], allow_slow_non_contiguous=True)
    K.dma("sp", dtbb.ap, dram_ap(dtb_d, 0, [[0, 128], [1, 64]]), writes=[dtbb.res])
    K.dma("sp", cdft.ap, cdft_d.ap()[:, :], writes=[cdft.res])
    for t_, w_ in ((prows[0], HW), (prows[1], HW), (prow_c, HCW)):
        K.op("dve", lambda e: e.memset(t_[:, 0:2], 0.0), writes=[t_.res])
        K.op("dve", lambda e: e.memset(t_[:, w_ - 2:w_], 0.0), writes=[t_.res])
    ctr = {"wb": 0, "tmp": 0, "stg": 0, "u": 0, "v": 0, "pc": 0}

    def load_w(col0, ncols=512):
        w = Wb[ctr["wb"] % 2]; ctr["wb"] += 1
        K.dma("pool", w.v(0, [[512, 8], [1, ncols]]), dram_ap(win_d, col0, [[INC, 128], [128 * INC, 8], [1, ncols]]), writes=[w.res])
        return w

    def tile_src(tt):
        return (hcT, (tt - NT) * 128) if tt >= NT else (hT, tt * 128)

    def conv_taps(acc_t, row_t, n, ci):
        K.op("dve", lambda e: e.tensor_scalar(out=acc_t.ap, in0=row_t[:, 0:n], scalar1=cwT[:, ci, 0:1], scalar2=None, op0=ALU.mult),
             reads=[row_t.res, cwT.res], writes=[acc_t.res])
        for j in range(1, 5):
            K.op("dve", lambda e: e.scalar_tensor_tensor(out=acc_t.ap, in0=row_t[:, j:j + n], scalar=cwT[:, ci, j:j + 1], in1=acc_t.ap, op0=ALU.mult, op1=ALU.add),
                 reads=[row_t.res, cwT.res, acc_t.res], writes=[acc_t.res])

    def conv_gen(w, cc, ci, tok_dst, tok_col, tok_width, tok_res, feat_dst, feat_row, feat_res, with_ctx):
        prow = prows[ctr["pc"] % 2]; ctr["pc"] += 1
        for tg in range(8):
            b = nextbank()
            for kc in range(8):
                K.mm(lambda pe: pe.matmul(pb[b][:, :], lhsT=w[:, kc, cc * 128:(cc + 1) * 128], rhs=hT[:, kc, 2 + tg * 512:2 + tg * 512 + 512], start=(kc == 0), stop=(kc == 7)),
                     reads=[hT.res, w.res], writes=[PB[b]], signal=(kc == 7))
            K.op("act", lambda e: e.copy(out=prow[:, 2 + tg * 512:2 + tg * 512 + 512], in_=pb[b][:, :]), reads=[PB[b]], writes=[prow.res])
        yield
        conv_taps(cacc, prow, SEQ, ci)
        K.op("act", lambda e: e.activation(out=xsT.ap, in_=cacc.ap, func=AF.Silu, bias=cbT[:, ci:ci + 1], scale=1.0), reads=[cacc.res, cbT.res], writes=[xsT.res])
        if feat_dst is not None:
            K.dma("sp", dram_ap(feat_dst, feat_row * SEQ, [[SEQ, 128], [1, SEQ]]), xsT.ap, reads=[xsT.res], writes=[feat_res])
        if tok_dst is not None:
            for t8_ in range(4):
                b = nextbank()
                for i8 in range(8):
                    tt = t8_ * 8 + i8
                    K.mm(lambda pe: pe.transpose(pbb[b][:, i8 * 128:(i8 + 1) * 128], xsT[:, tt * 128:(tt + 1) * 128], ident_b.ap),
                         reads=[xsT.res, ident_b.res], writes=[PB[b]], signal=(i8 == 7))
                K.op("act", lambda e: e.copy(out=tokbuf.v(t8_ * 8 * 128, [[1, 1024]]), in_=pbb[b][:, 0:1024]), reads=[PB[b]], writes=[tokbuf.res])
        if with_ctx:
            b = nextbank()
            for kc in range(8):
                K.mm(lambda pe: pe.matmul(pb[b][:, 0:NCTX], lhsT=w[:, kc, cc * 128:(cc + 1) * 128], rhs=hcT[:, kc, 2:2 + NCTX], start=(kc == 0), stop=(kc == 7)),
                     reads=[hcT.res, w.res], writes=[PB[b]], signal=(kc == 7))
            K.op("act", lambda e: e.copy(out=prow_c[:, 2:2 + NCTX], in_=pb[b][:, 0:NCTX]), reads=[PB[b]], writes=[prow_c.res])
            conv_taps(cacc_c, prow_c, NCTX, ci)
            K.op("act", lambda e: e.activation(out=xsT_c.ap, in_=cacc_c.ap, func=AF.Silu, bias=cbT[:, ci:ci + 1], scale=1.0), reads=[cacc_c.res, cbT.res], writes=[xsT_c.res])
            b = nextbank()
            for i8 in range(2):
                K.mm(lambda pe: pe.transpose(pbb[b][:, i8 * 128:(i8 + 1) * 128], xsT_c[:, i8 * 128:(i8 + 1) * 128], ident_b.ap),
                     reads=[xsT_c.res, ident_b.res], writes=[PB[b]], signal=(i8 == 1))
            K.op("act", lambda e: e.copy(out=tokbuf.v(NT * 128, [[1, 256]]), in_=pbb[b][:, 0:256]), reads=[PB[b]], writes=[tokbuf.res])
        if tok_dst is not None:
            for hf in range(2):
                K.dma("sp", dram_ap(tok_dst, hf * 17 * 128 * tok_width + tok_col, [[tok_width, 128], [128 * tok_width, 17], [1, 128]]),
                      tokbuf.v(hf * 17 * 128, [[128, 17], [1, 128]]), reads=[tokbuf.res], writes=[tok_res[hf]])

    def run_pipelined(gens):
        prev = None
        for g_ in gens:
            next(g_)
            if prev is not None:
                for _ in prev:
                    pass
            prev = g_
        if prev is not None:
            for _ in prev:
                pass

    def tok_plain(w, func, dst_d, dcol0, dwidth, dres):
        for tt in range(NT):
            b = nextbank()
            for kc in range(8):
                K.mm(lambda pe: pe.matmul(pb[b][:, :], lhsT=hT[:, kc, 2 + tt * 128:2 + tt * 128 + 128], rhs=w[:, kc, :], start=(kc == 0), stop=(kc == 7)),
                     reads=[hT.res, w.res], writes=[PB[b]], signal=(kc == 7))
            sg = stg[ctr["stg"] % 4]; ctr["stg"] += 1
            K.op("act", lambda e: e.activation(out=sg.ap, in_=pb[b][:, :], func=func), reads=[PB[b]], writes=[sg.res])
            K.dma("sp", dram_ap(dst_d, tt * 128 * dwidth + dcol0, [[dwidth, 128], [1, 512]]), sg.ap, reads=[sg.res], writes=[dres])

    def xb_chunks():
        for blk in range(4):
            w = load_w(blk * 512)
            for cc in range(4):
                yield conv_gen(w, cc, blk * 4 + cc, XS, blk * 512 + cc * 128, DI, R_XS, None, 0, None, True)
        for d_ in range(2):
            w = load_w(OFF_BF + d_ * 512)
            for cc in range(4):
                yield conv_gen(w, cc, 16 + d_ * 4 + cc, BTOK, d_ * 512 + cc * 128, 1024, R_BTOK, BT, d_ * 512 + cc * 128, R_BT, True)
    run_pipelined(xb_chunks())
    w = load_w(OFF_DT, 64)
    for tt in range(NT + NTC):
        src, t0 = tile_src(tt)
        b = nextbank()
        for kc in range(8):
            K.mm(lambda pe: pe.matmul(pb[b][:, 0:64], lhsT=src[:, kc, 2 + t0:2 + t0 + 128], rhs=w[:, kc, 0:64], start=(kc == 0), stop=(kc == 7)),
                 reads=[src.res, w.res], writes=[PB[b]], signal=(kc == 7))
        tf = tmpf[ctr["tmp"] % 2]; ctr["tmp"] += 1
        K.op("dve", lambda e: e.tensor_tensor(out=tf[:, 0:64], in0=pb[b][:, 0:64], in1=dtbb.ap, op=ALU.add), reads=[PB[b], dtbb.res], writes=[tf.res])
        K.op("act", lambda e: e.activation(out=tf[:, 0:64], in_=tf[:, 0:64], func=AF.Exp), reads=[tf.res], writes=[tf.res])
        K.op("act", lambda e: e.activation(out=dtS[:, tt, :], in_=tf[:, 0:64], func=AF.Ln, bias=1.0, scale=1.0), reads=[tf.res], writes=[dtS.res])
    def c_chunks():
        for d_ in range(2):
            w = load_w(OFF_CF + d_ * 512)
            for cc in range(4):
                yield conv_gen(w, cc, 24 + d_ * 4 + cc, None, 0, 0, None, CT, d_ * 512 + cc * 128, R_CT, False)
    run_pipelined(c_chunks())
    for blk in range(4):
        w = load_w(OFF_Z + blk * 512)
        tok_plain(w, AF.Silu, SZ, blk * 512, DI, R_SZ)
    for blk in range(2):
        w = load_w(OFF_F + blk * 512)
        for g in range(4):
            gg = blk * 4 + g
            for tg in range(8):
                b = nextbank()
                for kc in range(8):
                    K.mm(lambda pe: pe.matmul(pb[b][:, :], lhsT=w[:, kc, g * 128:(g + 1) * 128], rhs=hT[:, kc, 2 + tg * 512:2 + tg * 512 + 512],
                                              start=(kc == 0), stop=(kc == 7)), reads=[hT.res, w.res], writes=[PB[b]], signal=(kc == 7))
                u = uT[ctr["u"] % 2]; ctr["u"] += 1
                K.op("act", lambda e: e.copy(out=u.ap, in_=pb[b][:, :]), reads=[PB[b]], writes=[u.res])
                b2 = nextbank()
                b3 = nextbank()
                for sub in range(4):
                    bb_ = b2 if sub < 2 else b3
                    K.mm(lambda pe: pe.matmul(pb[bb_][:, (sub % 2) * 256:(sub % 2) * 256 + 256], lhsT=u[:, sub * 128:(sub + 1) * 128], rhs=cdft.ap, start=True, stop=True),
                         reads=[u.res, cdft.res], writes=[PB[bb_]], signal=(sub % 2 == 1))
                vs_ = vst[ctr["v"] % 2]; ctr["v"] += 1
                K.op("dve", lambda e: e.tensor_copy(out=vs_.v(0, [[1, 512]]), in_=pb[b2][:, :]), reads=[PB[b2]], writes=[vs_.res])
                K.op("dve", lambda e: e.tensor_copy(out=vs_.v(512, [[1, 512]]), in_=pb[b3][:, :]), reads=[PB[b3]], writes=[vs_.res])
                K.dma("sp", dram_ap(V, tg * 512 * 2048 + gg * 256, [[2048, 128], [128 * 2048, 4], [1, 256]]), vs_.ap, reads=[vs_.res], writes=[R_V])
    for blk in range(4):
        w = load_w(OFF_GF + blk * 512)
        tok_plain(w, AF.Sigmoid, G, blk * 512, 2 * D, R_G)
    if debug:
        dbg["dtS"] = nc.dram_tensor("dbg_dtS", [128, (NT + NTC) * 64], F32, kind="ExternalOutput")
        K.dma("sp", dbg["dtS"].ap()[:, :], dtS.v(0, [[1, (NT + NTC) * 64]]), reads=[dtS.res], writes=[Res()])
    K.barrier()
    A.release(persist_mark)
    if stop_after == "p1b":
        return nc, dbg, K, A

    m3 = A.mark()
    Vs = A.alloc([128, 32, 4, 256], BF16, "Vs")
    R_Vs = [Res() for _ in range(32)]
    ctb = [A.alloc([128, 32, 256], BF16, f"ctb{i}") for i in range(2)]
    nsb = [A.alloc([128, 32, 256], BF16, f"nsb{i}") for i in range(2)]
    uos = [A.alloc([128, 256], BF16, f"uos{i}") for i in range(4)]
    fscale = float(1.0 / np.sqrt(4096.0 * 128.0))
    nuo = 0
    for hh in range(2):
        for tc in range(32):
            K.dma("sp", Vs.v(tc * 1024, [[1, 1024]]), dram_ap(V, tc * 128 * 2048 + hh * 1024, [[2048, 128], [1, 1024]]), writes=[R_Vs[tc]])
        for kg in range(16):
            cb_ = ctb[kg % 2]; nb_ = nsb[kg % 2]
            K.dma("sp", cb_.v(0, [[1, 8192]]), dram_ap(ct_d, kg * 128 * 8192, [[8192, 128], [1, 8192]]), writes=[cb_.res])
            K.dma("sp", nb_.v(0, [[1, 8192]]), dram_ap(nst_d, kg * 128 * 8192, [[8192, 128], [1, 8192]]), writes=[nb_.res])
            for g in range(4):
                b = nextbank()
                for tc in range(32):
                    K.mm(lambda pe: pe.matmul(pb[b][:, 0:256], lhsT=Vs[:, tc, g, 0:128], rhs=cb_[:, tc, :], start=(tc == 0), stop=False),
                         reads=[R_Vs[tc], cb_.res], writes=[PB[b]], signal=False)
                    K.mm(lambda pe: pe.matmul(pb[b][:, 0:256], lhsT=Vs[:, tc, g, 128:256], rhs=nb_[:, tc, :], start=False, stop=(tc == 31)),
                         reads=[R_Vs[tc], nb_.res], writes=[PB[b]], signal=(tc == 31))
                u = uos[nuo % 4]; nuo += 1
                K.op("act", lambda e: e.activation(out=u.ap, in_=pb[b][:, 0:256], func=AF.Copy, scale=fscale), reads=[PB[b]], writes=[u.res])
                K.dma("sp", dram_ap(UOT, ((hh * 4 + g) * 128) * SEQ + kg * 256, [[SEQ, 128], [1, 256]]), u.ap, reads=[u.res], writes=[R_UOT])
    K.barrier()
    A.release(m3)
    m3b = A.mark()
    Wfo = A.alloc([128, 8, D], BF16, "Wfo")
    uo_c = [A.alloc([128, 8, 128], BF16, f"uo_c{i}") for i in range(2)]
    yfs = [A.alloc([128, D], F32, f"yfs{i}") for i in range(2)]
    K.dma("pool", Wfo.ap, dram_ap(wfo_d, 0, [[D, 128], [128 * D, 8], [1, D]]), writes=[Wfo.res])
    for c in range(NT):
        uo = uo_c[c % 2]; yf_ = yfs[c % 2]
        K.dma("sp", uo.ap, dram_ap(UOT, c * 128, [[SEQ, 128], [128 * SEQ, 8], [1, 128]]), writes=[uo.res])
        for dh in range(2):
            b = nextbank()
            for k in range(8):
                K.mm(lambda pe: pe.matmul(pb[b][:, :], lhsT=uo[:, k, :], rhs=Wfo[:, k, dh * 512:(dh + 1) * 512], start=(k == 0), stop=(k == 7)),
                     reads=[uo.res, Wfo.res], writes=[PB[b]], signal=(k == 7))
            K.op("act", lambda e: e.copy(out=yf_[:, dh * 512:(dh + 1) * 512], in_=pb[b][:, :]), reads=[PB[b]], writes=[yf_.res])
        K.dma("sp", dram_ap(YFOUR, c * 128 * D, [[D, 128], [1, D]]), yf_.ap, reads=[yf_.res], writes=[R_YFOUR])
    K.barrier()
    A.release(m3b)
    if stop_after == "p3":
        return nc, dbg, K, A

    dsk_b = A.alloc([128, NH], F32, "dsk_b")
    K.dma("sp", dsk_b.ap, dram_ap(dsk_d, 0, [[0, 128], [1, NH]]), writes=[dsk_b.res])
    Hst = [A.alloc([128, DI], F32, f"H{d_}") for d_ in range(2)]
    for d_ in range(2):
        K.op("dve", lambda e: e.memset(Hst[d_].ap, 0.0), writes=[Hst[d_].res])
    m2 = A.mark()

    def ssd_bufs(nb):
        B_ = {}
        B_["nb"] = nb
        B_["xs"] = [A.alloc([128, DI], BF16, "xs_c") for _ in range(nb)]
        B_["btok"] = [A.alloc([128, 512], BF16, "btok_c") for _ in range(nb)]
        B_["bt"] = [A.alloc([128, 4, 128], BF16, "bt_c") for _ in range(nb)]
        B_["ct"] = [A.alloc([128, 4, 128], BF16, "ct_c") for _ in range(nb)]
        B_["dta"] = [A.alloc([128, 32], F32, "dta") for _ in range(2)]
        B_["E"] = [A.alloc([128, 96], F32, "E") for _ in range(2)]
        B_["rseg"] = [A.alloc([128, 512], F32, "rseg") for _ in range(nb)]
        B_["LT"] = [A.alloc([128, 512], F32, "LT") for _ in range(nb)]
        B_["CBm"] = A.alloc([128, 512], F32, "CBm")
        B_["MT"] = [A.alloc([128, 32, 128], BF16, "MT") for _ in range(nb)]
        B_["xdt"] = [A.alloc([128, DI], BF16, "xdt") for _ in range(nb)]
        B_["xdd"] = [A.alloc([128, DI], BF16, "xdd") for _ in range(1)]
        B_["Hb"] = [A.alloc([128, DI], BF16, "Hb") for _ in range(1)]
        B_["yoff"] = [A.alloc([128, 512], F32, "yoff") for _ in range(2)]
        B_["ysb"] = [A.alloc([128, DI], F32, "ysb") for _ in range(nb)]
        B_["i"] = 0
        return B_

    def ssd_step(B_, d_, c, row, want_y, after_loads=None):
        i = B_["i"]; B_["i"] += 1
        par = i % B_["nb"]; p2 = i % 2
        incl = triU if d_ == 0 else triL
        excl = striL if d_ == 0 else striU
        valid = incl
        xs_ = B_["xs"][par]; btok_ = B_["btok"][par]; bt_ = B_["bt"][par]; ct_ = B_["ct"][par]
        dta = B_["dta"][p2]; E = B_["E"][p2]; MT = B_["MT"][par]; xdt = B_["xdt"][par]; xdd = B_["xdd"][0]; Hb = B_["Hb"][0]
        ysb = B_["ysb"][par]; CBm = B_["CBm"]; H = Hst[d_]
        K.dma("sp", xs_.ap, dram_ap(XS, row * 128 * DI, [[DI, 128], [1, DI]]), writes=[xs_.res])
        K.dma("sp", btok_.ap, dram_ap(BTOK, row * 128 * 1024 + d_ * 512, [[1024, 128], [1, 512]]), writes=[btok_.res])
        if want_y:
            K.dma("sp", bt_.ap, dram_ap(BT, d_ * 512 * SEQ + c * 128, [[SEQ, 128], [128 * SEQ, 4], [1, 128]]), writes=[bt_.res])
            K.dma("sp", ct_.ap, dram_ap(CT, d_ * 512 * SEQ + c * 128, [[SEQ, 128], [128 * SEQ, 4], [1, 128]]), writes=[ct_.res])
        if after_loads is not None:
            after_loads()
        dtoff = row * 64 + d_ * 32
        K.op("dve", lambda e: e.tensor_tensor(out=dta.ap, in0=dtS.v(dtoff, [[1, 32]]), in1=a_b[:, d_ * 32:(d_ + 1) * 32], op=ALU.mult),
             reads=[dtS.res, a_b.res], writes=[dta.res])
        for n_, lt_ in enumerate((incl, excl, ones_f)):
            K.mm(lambda pe: pe.matmul(pb[0][:, n_ * 32:(n_ + 1) * 32], lhsT=lt_.ap, rhs=dta.ap, start=True, stop=True),
                 reads=[lt_.res, dta.res], writes=[PB[0]], signal=(n_ == 2))
        K.op("act", lambda e: e.activation(out=E.ap, in_=pb[0][:, 0:96], func=AF.Exp), reads=[PB[0]], writes=[E.res])
        if want_y:
            for g in range(4):
                K.mm(lambda pe: pe.matmul(pb[1][:, g * 128:(g + 1) * 128], lhsT=bt_[:, g, :], rhs=ct_[:, g, :], start=True, stop=True),
                     reads=[bt_.res, ct_.res], writes=[PB[1]], signal=(g == 3))
            K.op("dve", lambda e: e.tensor_tensor(out=CBm.v(0, [[128, 4], [1, 128]]), in0=AP(pb[1], 0, [[512, 128], [128, 4], [1, 128]]),
                                                  in1=valid.v(0, [[0, 4], [1, 128]]), op=ALU.mult), reads=[PB[1], valid.res], writes=[CBm.res])
            for q in range(8):
                rs_ = B_["rseg"][q % B_["nb"]]; l_ = B_["LT"][q % B_["nb"]]; bk = 2 + q % 2
                K.op("pool", lambda e: e.tensor_tensor(out=rs_.v(0, [[128, 4], [1, 128]]), in0=incl.v(0, [[0, 4], [1, 128]]),
                                                       in1=dta.v(4 * q, [[1, 4], [0, 128]]), op=ALU.mult), reads=[incl.res, dta.res], writes=[rs_.res])
                K.mm(lambda pe: pe.matmul(pb[bk][:, :], lhsT=excl.ap, rhs=rs_.ap, start=True, stop=True), reads=[excl.res, rs_.res], writes=[PB[bk]])
                K.op("act", lambda e: e.activation(out=l_.ap, in_=pb[bk][:, :], func=AF.Exp), reads=[PB[bk]], writes=[l_.res])
                g = q // 2
                K.op("dve", lambda e: e.tensor_tensor(out=MT.v(4 * q * 128, [[128, 4], [1, 128]]), in0=l_.v(0, [[128, 4], [1, 128]]),
                                                      in1=CBm.v(g * 128, [[0, 4], [1, 128]]), op=ALU.mult), reads=[l_.res, CBm.res], writes=[MT.res])
        K.op("dve", lambda e: e.tensor_tensor(out=xdt.v(0, [[64, 32], [1, 64]]), in0=xs_.v(0, [[64, 32], [1, 64]]),
                                              in1=dtS.v(dtoff, [[1, 32], [0, 64]]), op=ALU.mult), reads=[xs_.res, dtS.res], writes=[xdt.res])
        if want_y:
            K.op("act", lambda e: e.copy(out=Hb.ap, in_=H.ap), reads=[H.res], writes=[Hb.res])
            for g in range(4):
                bd = 4 + g % 2
                yo = B_["yoff"][g % 2]
                for hh in range(8):
                    h = g * 8 + hh
                    K.mm(lambda pe: pe.matmul(pb[bd][:, hh * 64:(hh + 1) * 64], lhsT=MT[:, h, :], rhs=xdt[:, h * 64:(h + 1) * 64], start=True, stop=True),
                         reads=[MT.res, xdt.res], writes=[PB[bd]], signal=(hh == 7))
                K.mm(lambda pe: pe.matmul(pb[6][:, :], lhsT=ct_[:, g, :], rhs=Hb[:, g * 512:(g + 1) * 512], start=True, stop=True),
                     reads=[ct_.res, Hb.res], writes=[PB[6]])
                K.op("dve", lambda e: e.tensor_tensor(out=yo.v(0, [[64, 8], [1, 64]]), in0=AP(pb[6], 0, [[512, 128], [64, 8], [1, 64]]),
                                                      in1=E.v(g * 8, [[1, 8], [0, 64]]), op=ALU.mult), reads=[PB[6], E.res], writes=[yo.res])
                K.op("dve", lambda e: e.tensor_tensor(out=ysb[:, g * 512:(g + 1) * 512], in0=pb[bd][:, :], in1=yo.ap, op=ALU.add),
                     reads=[PB[bd], yo.res], writes=[ysb.res])
        K.op("dve", lambda e: e.tensor_tensor(out=xdd.v(0, [[64, 32], [1, 64]]), in0=xdt.v(0, [[64, 32], [1, 64]]),
                                              in1=E.v(32, [[1, 32], [0, 64]]), op=ALU.mult), reads=[xdt.res, E.res], writes=[xdd.res])
        for g in range(4):
            K.mm(lambda pe: pe.matmul(pb[7][:, :], lhsT=btok_[:, g * 128:(g + 1) * 128], rhs=xdd[:, g * 512:(g + 1) * 512], start=True, stop=True),
                 reads=[btok_.res, xdd.res], writes=[PB[7]])
            K.op("dve", lambda e: e.tensor_tensor(out=H.v(g * 512, [[64, 8], [1, 64]]), in0=H.v(g * 512, [[64, 8], [1, 64]]),
                                                  in1=E.v(64 + g * 8, [[1, 8], [0, 64]]), op=ALU.mult), reads=[H.res, E.res], writes=[H.res])
            K.op("dve", lambda e: e.tensor_tensor(out=H[:, g * 512:(g + 1) * 512], in0=H[:, g * 512:(g + 1) * 512], in1=pb[7][:, :], op=ALU.add),
                 reads=[H.res, PB[7]], writes=[H.res])
        return xs_, ysb

    SB = ssd_bufs(2)
    ssd_step(SB, 0, 0, NT + 0, False)
    ssd_step(SB, 0, 1, NT + 1, False)
    ssd_step(SB, 1, 1, NT + 1, False)
    ssd_step(SB, 1, 0, NT + 0, False)
    if debug:
        dbg["Hctx"] = nc.dram_tensor("dbg_Hctx", [2, 128, DI], F32, kind="ExternalOutput")
        for d_ in range(2):
            K.dma("sp", dram_ap(dbg["Hctx"], d_ * 128 * DI, [[DI, 128], [1, DI]]), Hst[d_].ap, reads=[Hst[d_].res], writes=[Res()])
    for c in range(NT):
        _, ysb = ssd_step(SB, 0, c, c, True)
        K.dma("sp", dram_ap(YF, c * 128 * DI, [[DI, 128], [1, DI]]), ysb.ap, reads=[ysb.res], writes=[R_YF[c]])
    K.barrier()
    A.release(m2)
    if stop_after == "p2a":
        return nc, dbg, K, A

    def subtile(parent, eoff, shape, dt, name):
        t = Tile(A, 0, shape, dt, name)
        esz_p = 4 if parent.dt == F32 else 2
        esz = 4 if dt == F32 else 2
        t.base = (parent.base * esz_p + eoff * esz_p) // esz
        dims = [[t.ps, shape[0]]]
        st = int(np.prod(shape[1:]))
        for d__ in shape[1:]:
            st //= d__
            dims.append([st, d__])
        t.ap = AP(t.h, t.base, dims)
        return t

    x_c = subtile(modb, 0, [128, D], F32, "x_c")
    yfour_c = subtile(modb, D, [128, D], F32, "yfour_c")
    m1 = subtile(modb, 5 * D, [128, D], F32, "m1")
    h2f = m1
    g1b = modb[:, 2 * D:3 * D]; sh2b = modb[:, 3 * D:4 * D]; sc2b = modb[:, 4 * D:5 * D]
    Wso = A.alloc([128, 16, D], BF16, "Wso"); Wo = A.alloc([128, 8, D], BF16, "Wo"); wr = A.alloc([128, 8, NE], F32, "wr")
    R_Wso = [Res() for _ in range(4)]
    snw_b = A.alloc([128, DI], F32, "snw_b"); brb = A.alloc([128, NE], F32, "brb")
    big = A.alloc([128, DI], F32, "big")
    sz_c = A.alloc([128, DI], BF16, "sz_c"); g_c = A.alloc([128, 2 * D], BF16, "g_c")
    gzn = A.alloc([128, DI], BF16, "gzn"); gznT = A.alloc([128, 16, 128], BF16, "gznT")
    merged = A.alloc([128, D], BF16, "merged"); mergedT = A.alloc([128, 8, 128], BF16, "mergedT")
    x1t = A.alloc([128, D], F32, "x1t"); h2T32 = A.alloc([128, 8, 128], F32, "h2T32")
    ssq = A.alloc([128, NT], F32, "ssq"); rq = A.alloc([128, NT], F32, "rq"); rstdq = A.alloc([128, NT], F32, "rstdq")
    ss2 = A.alloc([128, NT], F32, "ss2"); r2 = A.alloc([128, NT], F32, "r2"); rstd2 = A.alloc([128, NT], F32, "rstd2")
    lg = A.alloc([128, NE], F32, "lg"); t8 = A.alloc([128, 8], F32, "t8"); msk = A.alloc([128, NE], F32, "msk")
    negm = A.alloc([128, 1], F32, "negm"); ex = A.alloc([128, NE], F32, "ex"); ssum = A.alloc([128, 1], F32, "ssum"); rsum = A.alloc([128, 1], F32, "rsum")
    for i4 in range(4):
        K.dma("pool", Wso.v(i4 * 4 * D, [[D, 4], [1, D]]), dram_ap(wso_d, i4 * 512 * D, [[D, 128], [128 * D, 4], [1, D]]), writes=[R_Wso[i4]])
    K.dma("pool", Wo.ap, dram_ap(wo_d, 0, [[D, 128], [128 * D, 8], [1, D]]), writes=[Wo.res])
    K.dma("sp", wr.ap, dram_ap(wr_d, 0, [[NE, 128], [128 * NE, 8], [1, NE]]), writes=[wr.res])
    K.dma("sp", snw_b.ap, dram_ap(snw_d, 0, [[0, 128], [1, DI]]), writes=[snw_b.res])
    K.dma("sp", brb.ap, dram_ap(br_d, 0, [[0, 128], [1, NE]]), writes=[brb.res])
    SB1 = ssd_bufs(1)
    rows = lambda t_, c_, w_: dram_ap(t_, c_ * 128 * w_, [[w_, 128], [1, w_]])

    def finish_loads(c):
        K.dma("sp", big.ap, rows(YF, c, DI), reads=[R_YF[c]], writes=[big.res])
        K.dma("sp", sz_c.ap, rows(SZ, c, DI), writes=[sz_c.res])
        K.dma("sp", g_c.ap, rows(G, c, 2 * D), writes=[g_c.res])
        K.dma("sp", yfour_c.ap, rows(YFOUR, c, D), writes=[yfour_c.res])
        K.dma("sp", x_c.ap, rows(x_d, c, D), writes=[x_c.res])

    def finish(c, xs_, ysb):
        K.op("dve", lambda e: e.tensor_tensor(out=ysb.ap, in0=ysb.ap, in1=big.ap, op=ALU.add), reads=[ysb.res, big.res], writes=[ysb.res])
        K.op("dve", lambda e: e.tensor_tensor(out=big.v(0, [[64, 32], [1, 64]]), in0=xs_.v(0, [[64, 32], [1, 64]]), in1=dsk_b.v(0, [[1, 32], [0, 64]]), op=ALU.mult),
             reads=[xs_.res, dsk_b.res], writes=[big.res])
        K.op("dve", lambda e: e.tensor_tensor(out=ysb.ap, in0=ysb.ap, in1=big.ap, op=ALU.add), reads=[ysb.res, big.res], writes=[ysb.res])
        if debug and c == NT - 1:
            dbg["ylast"] = nc.dram_tensor("dbg_ylast", [128, DI], F32, kind="ExternalOutput")
            K.dma("sp", dbg["ylast"].ap()[:, :], ysb.ap, reads=[ysb.res], writes=[Res()])
        K.op("dve", lambda e: e.tensor_tensor(out=big.ap, in0=ysb.ap, in1=sz_c.ap, op=ALU.mult), reads=[ysb.res, sz_c.res], writes=[big.res])
        K.op("act", lambda e: e.activation(out=gzn.ap, in_=big.ap, func=AF.Square, accum_out=ssq[:, c:c + 1]), reads=[big.res], writes=[gzn.res, ssq.res])
        K.op("act", lambda e: e.activation(out=rq[:, c:c + 1], in_=ssq[:, c:c + 1], func=AF.Sqrt, bias=epsT[:, 0:1], scale=1.0 / DI), reads=[ssq.res, epsT.res], writes=[rq.res])
        K.op("dve", lambda e: e.reciprocal(out=rstdq[:, c:c + 1], in_=rq[:, c:c + 1]), reads=[rq.res], writes=[rstdq.res])
        K.op("dve", lambda e: e.scalar_tensor_tensor(out=gzn.ap, in0=big.ap, scalar=rstdq[:, c:c + 1], in1=snw_b.ap, op0=ALU.mult, op1=ALU.mult),
             reads=[big.res, rstdq.res, snw_b.res], writes=[gzn.res])
        for f in range(16):
            K.mm(lambda pe: pe.transpose(pbb[f // 8][:, (f % 8) * 128:(f % 8 + 1) * 128], gzn[:, f * 128:(f + 1) * 128], ident_b.ap),
                 reads=[gzn.res, ident_b.res], writes=[PB[f // 8]], signal=(f % 8 == 7))
        for hf in range(2):
            K.op("act", lambda e: e.copy(out=gznT.v(hf * 1024, [[1, 1024]]), in_=pbb[hf][:, 0:1024]), reads=[PB[hf]], writes=[gznT.res])
        for dh in range(2):
            for f in range(16):
                K.mm(lambda pe: pe.matmul(pb[2 + dh][:, :], lhsT=gznT[:, f, :], rhs=Wso[:, f, dh * 512:(dh + 1) * 512], start=(f == 0), stop=(f == 15)),
                     reads=[gznT.res, R_Wso[f // 4]], writes=[PB[2 + dh]], signal=(f == 15))
            K.op("dve", lambda e: e.tensor_tensor(out=m1[:, dh * 512:(dh + 1) * 512], in0=pb[2 + dh][:, :], in1=g_c[:, D + dh * 512:D + (dh + 1) * 512], op=ALU.mult),
                 reads=[PB[2 + dh], g_c.res], writes=[m1.res])
        K.op("dve", lambda e: e.tensor_tensor(out=yfour_c.ap, in0=yfour_c.ap, in1=g_c[:, 0:D], op=ALU.mult), reads=[yfour_c.res, g_c.res], writes=[yfour_c.res])
        K.op("dve", lambda e: e.tensor_tensor(out=merged.ap, in0=m1.ap, in1=yfour_c.ap, op=ALU.add), reads=[m1.res, yfour_c.res], writes=[merged.res])
        for k in range(8):
            K.mm(lambda pe: pe.transpose(pbb[6][:, k * 128:(k + 1) * 128], merged[:, k * 128:(k + 1) * 128], ident_b.ap),
                 reads=[merged.res, ident_b.res], writes=[PB[6]], signal=(k == 7))
        K.op("act", lambda e: e.copy(out=mergedT.v(0, [[1, 1024]]), in_=pbb[6][:, 0:1024]), reads=[PB[6]], writes=[mergedT.res])
        for dh in range(2):
            for k in range(8):
                K.mm(lambda pe: pe.matmul(pb[4 + dh][:, :], lhsT=mergedT[:, k, :], rhs=Wo[:, k, dh * 512:(dh + 1) * 512], start=(k == 0), stop=(k == 7)),
                     reads=[mergedT.res, Wo.res], writes=[PB[4 + dh]], signal=(k == 7))
            K.op("dve", lambda e: e.tensor_tensor(out=x1t[:, dh * 512:(dh + 1) * 512], in0=pb[4 + dh][:, :], in1=g1b[:, dh * 512:(dh + 1) * 512], op=ALU.mult),
                 reads=[PB[4 + dh], modb.res], writes=[x1t.res])
        K.op("dve", lambda e: e.tensor_tensor(out=x1t.ap, in0=x1t.ap, in1=x_c.ap, op=ALU.add), reads=[x1t.res, x_c.res], writes=[x1t.res])
        K.dma("sp", rows(X1, c, D), x1t.ap, reads=[x1t.res], writes=[R_X1[c]])
        K.op("act", lambda e: e.activation(out=gzn[:, 0:D], in_=x1t.ap, func=AF.Square, accum_out=ss2[:, c:c + 1]), reads=[x1t.res], writes=[gzn.res, ss2.res])
        K.op("act", lambda e: e.activation(out=r2[:, c:c + 1], in_=ss2[:, c:c + 1], func=AF.Sqrt, bias=epsT[:, 0:1], scale=1.0 / D), reads=[ss2.res, epsT.res], writes=[r2.res])
        K.op("dve", lambda e: e.reciprocal(out=rstd2[:, c:c + 1], in_=r2[:, c:c + 1]), reads=[r2.res], writes=[rstd2.res])
        K.op("dve", lambda e: e.scalar_tensor_tensor(out=h2f.ap, in0=x1t.ap, scalar=rstd2[:, c:c + 1], in1=sc2b, op0=ALU.mult, op1=ALU.mult),
             reads=[x1t.res, rstd2.res, modb.res], writes=[h2f.res])
        K.op("dve", lambda e: e.tensor_tensor(out=h2f.ap, in0=h2f.ap, in1=sh2b, op=ALU.add), reads=[h2f.res, modb.res], writes=[h2f.res])
        for k in range(8):
            K.mm(lambda pe: pe.transpose(pb[k // 4][:, (k % 4) * 128:(k % 4 + 1) * 128], h2f[:, k * 128:(k + 1) * 128], ident_f.ap),
                 reads=[h2f.res, ident_f.res], writes=[PB[k // 4]], signal=(k % 4 == 3))
        for hf in range(2):
            K.op("act", lambda e: e.copy(out=h2T32.v(hf * 512, [[1, 512]]), in_=pb[hf][:, :]), reads=[PB[hf]], writes=[h2T32.res])
        K.op("act", lambda e: e.copy(out=merged.ap, in_=h2f.ap), reads=[h2f.res], writes=[merged.res])
        K.dma("sp", dram_ap(H2TOK, c * 128 * 1088, [[1088, 128], [1, D]]), merged.ap, reads=[merged.res], writes=[R_H2T])
        for k in range(8):
            K.mm(lambda pe: pe.matmul(pb[7][:, 0:NE], lhsT=h2T32[:, k, :], rhs=wr[:, k, :], start=(k == 0), stop=(k == 7)),
                 reads=[h2T32.res, wr.res], writes=[PB[7]], signal=(k == 7))
        K.op("dve", lambda e: e.tensor_tensor(out=lg.ap, in0=pb[7][:, 0:NE], in1=brb.ap, op=ALU.add), reads=[PB[7], brb.res], writes=[lg.res])
        K.op("dve", lambda e: e.max(out=t8.ap, in_=lg.ap), reads=[lg.res], writes=[t8.res])
        K.op("dve", lambda e: e.tensor_scalar(out=msk.ap, in0=lg.ap, scalar1=t8[:, 3:4], scalar2=None, op0=ALU.is_ge), reads=[lg.res, t8.res], writes=[msk.res])
        K.mm(lambda pe: pe.matmul(pb[7][:, 32:64], lhsT=triU.ap, rhs=msk.ap, start=True, stop=True), reads=[triU.res, msk.res], writes=[PB[7]], signal=False)
        K.mm(lambda pe: pe.matmul(pb[7][:, 64:96], lhsT=ones_f.ap, rhs=msk.ap, start=True, stop=True), reads=[ones_f.res, msk.res], writes=[PB[7]])
        K.op("dve", lambda e: e.tensor_tensor(out=rkS[:, c, :], in0=pb[7][:, 32:64], in1=msk.ap, op=ALU.subtract), reads=[PB[7], msk.res], writes=[rkS.res])
        K.op("dve", lambda e: e.tensor_tensor(out=rkS[:, c, :], in0=rkS[:, c, :], in1=cnt_b.ap, op=ALU.add), reads=[rkS.res, cnt_b.res], writes=[rkS.res])
        K.op("dve", lambda e: e.tensor_tensor(out=cnt_b.ap, in0=cnt_b.ap, in1=pb[7][:, 64:96], op=ALU.add), reads=[cnt_b.res, PB[7]], writes=[cnt_b.res])
        K.op("dve", lambda e: e.tensor_scalar(out=negm.ap, in0=t8[:, 0:1], scalar1=-1.0, scalar2=None, op0=ALU.mult), reads=[t8.res], writes=[negm.res])
        K.op("act", lambda e: e.activation(out=ex.ap, in_=lg.ap, func=AF.Exp, bias=negm[:, 0:1], scale=1.0), reads=[lg.res, negm.res], writes=[ex.res])
        K.op("dve", lambda e: e.tensor_tensor(out=ex.ap, in0=ex.ap, in1=msk.ap, op=ALU.mult), reads=[ex.res, msk.res], writes=[ex.res])
        K.op("dve", lambda e: e.reduce_sum(out=ssum.ap, in_=ex.ap, axis=AX.X), reads=[ex.res], writes=[ssum.res])
        K.op("dve", lambda e: e.reciprocal(out=rsum.ap, in_=ssum.ap), reads=[ssum.res], writes=[rsum.res])
        K.op("dve", lambda e: e.tensor_scalar(out=wS[:, c, :], in0=ex.ap, scalar1=rsum[:, 0:1], scalar2=None, op0=ALU.mult), reads=[ex.res, rsum.res], writes=[wS.res])
        K.dma("sp", dram_ap(H2TOK, c * 128 * 1088 + D, [[1088, 128], [1, 64]]), wS[:, c, :].bitcast(BF16), reads=[wS.res], writes=[R_WTOK])

    for c in range(NT - 1, -1, -1):
        xs_, ysb = ssd_step(SB1, 1, c, c, True, after_loads=(lambda c_=c: finish_loads(c_)))
        finish(c, xs_, ysb)
    if debug:
        dbg["wS"] = nc.dram_tensor("dbg_wS", [128, NT * NE], F32, kind="ExternalOutput")
        K.dma("sp", dbg["wS"].ap()[:, :], wS.v(0, [[1, NT * NE]]), reads=[wS.res], writes=[Res()])
    K.barrier()
    A.release(moe_mark)
    if stop_after == "p2b":
        return nc, dbg, K, A

    IO = bass.IndirectOffsetOnAxis
    iv = lambda ap: ap.bitcast(I32)
    pstart_b = A.alloc([128, NE], F32, "pstart_b")
    iota4 = A.alloc([128, 4], F32, "iota4")
    m5 = A.mark()
    flT = A.alloc([128, NE, 8], F32, "flT"); fli = A.alloc([128, NE * 8], F32, "fli")
    nb_b = A.alloc([128, NE], F32, "nb_b"); d1 = A.alloc([128, NE], F32, "d1"); t32 = A.alloc([128, NE], F32, "t32"); t8b = A.alloc([128, 8], F32, "t8b"); p4 = A.alloc([128, 4], F32, "p4")
    io4i = A.alloc([128, 4], F32, "io4i")
    for j in range(8):
        K.op("dve", lambda e: e.tensor_scalar(out=flT.v(j, [[8, NE]]), in0=cnt_b.ap, scalar1=512.0 * j, scalar2=None, op0=ALU.is_gt), reads=[cnt_b.res], writes=[flT.res])
    K.op("dve", lambda e: e.reduce_sum(out=nb_b.ap, in_=flT.ap, axis=AX.X), reads=[flT.res], writes=[nb_b.res])
    K.op("dve", lambda e: e.memset(pstart_b[:, 0:1], 0.0), writes=[pstart_b.res])
    for ei in range(1, NE):
        K.op("dve", lambda e: e.tensor_tensor(out=pstart_b[:, ei:ei + 1], in0=pstart_b[:, ei - 1:ei], in1=nb_b[:, ei - 1:ei], op=ALU.add),
             reads=[pstart_b.res, nb_b.res], writes=[pstart_b.res])
    K.op("dve", lambda e: e.tensor_scalar(out=pstart_b.ap, in0=pstart_b.ap, scalar1=512.0, scalar2=None, op0=ALU.mult), reads=[pstart_b.res], writes=[pstart_b.res])
    K.op("dve", lambda e: e.tensor_copy(out=iv(fli.ap)[:, 0:NE], in_=nb_b.ap), reads=[nb_b.res, fli.res], writes=[fli.res])
    K.dma("sp", NBD.ap()[:, :], iv(fli.ap)[0:1, 0:NE], reads=[fli.res], writes=[R_FLAGS])
    K.op("pool", lambda e: e.iota(iv(io4i.ap), pattern=[[128, 4]], base=0, channel_multiplier=1), writes=[io4i.res])
    K.op("dve", lambda e: e.tensor_copy(out=iota4.ap, in_=iv(io4i.ap)), reads=[io4i.res], writes=[iota4.res])
    zt = A.alloc([128, 8 * 1088], BF16, "zt")
    h2w = [A.alloc([128, 1088], BF16, f"h2w{i}") for i in range(2)]
    K.op("pool", lambda e: e.memset(zt.ap, 0.0), writes=[zt.res])
    for i32 in range(32):
        K.dma("sp", dram_ap(H2SLOT, i32 * 1024 * 1088, [[8 * 1088, 128], [1, 8 * 1088]]), zt.ap, reads=[zt.res], writes=[Res()])
    K.barrier()
    for c in range(NT):
        K.op("dve", lambda e: e.scalar_tensor_tensor(out=d1.ap, in0=rkS[:, c, :], scalar=1.0, in1=pstart_b.ap, op0=ALU.add, op1=ALU.add),
             reads=[rkS.res, pstart_b.res], writes=[d1.res])
        K.op("dve", lambda e: e.tensor_scalar(out=t32.ap, in0=wS[:, c, :], scalar1=0.0, scalar2=None, op0=ALU.is_gt), reads=[wS.res], writes=[t32.res])
        K.op("dve", lambda e: e.tensor_tensor(out=d1.ap, in0=d1.ap, in1=t32.ap, op=ALU.mult), reads=[d1.res, t32.res], writes=[d1.res])
        K.op("dve", lambda e: e.max(out=t8b.ap, in_=d1.ap), reads=[d1.res], writes=[t8b.res])
        K.op("dve", lambda e: e.tensor_scalar(out=p4.ap, in0=t8b[:, 0:4], scalar1=-1.0, scalar2=0.0, op0=ALU.add, op1=ALU.max), reads=[t8b.res], writes=[p4.res])
        K.op("dve", lambda e: e.tensor_copy(out=iv(poskS[:, c, :]), in_=p4.ap), reads=[p4.res], writes=[poskS.res])
        hw_ = h2w[c % 2]
        K.dma("sp", hw_.ap, dram_ap(H2TOK, c * 128 * 1088, [[1088, 128], [1, 1088]]), writes=[hw_.res])
        for k in range(4):
            K.idma(out=H2SLOT.ap()[:, :], out_off=IO(ap=iv(poskS[:, c, k:k + 1]), axis=0), in_=hw_.ap, in_off=None, bc=NSLOT - 1,
                   reads=[poskS.res, hw_.res], writes=[Res()])
    if debug:
        dbg["cnt"] = nc.dram_tensor("dbg_cnt", [128, NE], F32, kind="ExternalOutput")
        K.dma("sp", dbg["cnt"].ap()[:, :], cnt_b.ap, reads=[cnt_b.res], writes=[Res()])
    K.barrier()
    A.release(m5)
    if stop_after == "p5a":
        return nc, dbg, K, A

    Wgu = [A.alloc([128, 8, 2 * D], BF16, f"Wgu{i}") for i in range(2)]
    Wd = [A.alloc([128, 8, D], BF16, f"Wd{i}") for i in range(2)]
    R_Wgu = [[Res() for _ in range(4)] for _ in range(2)]
    R_Wd = [[Res() for _ in range(2)] for _ in range(2)]
    bguT = A.alloc([128, 16, NE], F32, "bguT")
    m5b = A.mark()
    hg = A.alloc([128, 4, 1088], BF16, "hg"); R_hg = [Res() for _ in range(4)]
    h2cT = A.alloc([128, 8, 512], BF16, "h2cT")
    actT = [A.alloc([128, 8, 512], BF16, f"actT{i}") for i in range(2)]
    ys = A.alloc([128, 4, D], F32, "ys"); R_ys = [Res() for _ in range(4)]
    gtb = [A.alloc([128, 512], F32, f"gt{i}") for i in range(2)]; stb = [A.alloc([128, 512], F32, f"st{i}") for i in range(2)]
    utb = [A.alloc([128, 512], F32, f"ut{i}") for i in range(2)]
    nel = [0]
    posf = A.alloc([128, 4], F32, "posf"); posi = A.alloc([128, 4], F32, "posi")
    bgs = subtile(hg, 0, [NE, 2 * D], F32, "bgs")
    K.dma("sp", bgs.ap, bgu_d.ap()[:, :], writes=[bgs.res])
    for cch in range(16):
        b = nextbank()
        K.mm(lambda pe: pe.transpose(pb[b][:, 0:NE], bgs[:, cch * 128:(cch + 1) * 128], ident_f[0:NE, 0:NE]), reads=[bgs.res, ident_f.res], writes=[PB[b]])
        K.op("dve", lambda e: e.tensor_copy(out=bguT[:, cch, :], in_=pb[b][:, 0:NE]), reads=[PB[b]], writes=[bguT.res])
    bguT1 = A.alloc([128, 8, NE], F32, "bguT1")
    K.op("dve", lambda e: e.tensor_scalar(out=bguT1.ap, in0=bguT[:, 8:16, :], scalar1=1.0, scalar2=None, op0=ALU.add), reads=[bguT.res], writes=[bguT1.res])
    K.barrier()
    nact = 0
    ncp = 0
    wstage = [A.alloc([128, 8, 512], F32, f"wstage{i}") for i in range(2)]
    nstg = [0]

    def load_piece(e2, pc):
        stt = wstage[nstg[0] % 2]; nstg[0] += 1
        if pc < 4:
            src = dram_ap(wgu_d, e2 * D * 2 * D + pc * 512, [[2 * D, 128], [128 * 2 * D, 8], [1, 512]])
            dst = Wgu[e2 % 2].v(pc * 512, [[2 * D, 8], [1, 512]]); res_ = R_Wgu[e2 % 2][pc]
        else:
            src = dram_ap(wd_d, e2 * D * D + (pc - 4) * 512, [[D, 128], [128 * D, 8], [1, 512]])
            dst = Wd[e2 % 2].v((pc - 4) * 512, [[D, 8], [1, 512]]); res_ = R_Wd[e2 % 2][pc - 4]
        K.dma("sp", stt.ap, src, writes=[stt.res])
        K.op("act", lambda e: e.copy(out=dst, in_=stt.ap), reads=[stt.res], writes=[res_])

    for pc in range(6):
        load_piece(0, pc)
    for ei in range(n_exp):
        wg = Wgu[ei % 2]; wdn = Wd[ei % 2]; rg = R_Wgu[ei % 2]; rd = R_Wd[ei % 2]
        for j in range(8):
            K.guard_begin(NBD.ap()[0:1, ei:ei + 1], R_FLAGS, reload=(j == 0), thr=j)
            K.op("dve", lambda e: e.tensor_scalar(out=posf.ap, in0=iota4.ap, scalar1=pstart_b[:, ei:ei + 1], scalar2=float(j * 512), op0=ALU.add, op1=ALU.add),
                 reads=[iota4.res, pstart_b.res], writes=[posf.res])
            K.op("dve", lambda e: e.tensor_copy(out=iv(posi.ap), in_=posf.ap), reads=[posf.res], writes=[posi.res])
            for sub in range(4):
                K.idma(out=hg[:, sub, :], out_off=None, in_=H2SLOT.ap()[:, :], in_off=IO(ap=iv(posi[:, sub:sub + 1]), axis=0), bc=NSLOT - 1, reads=[posi.res], writes=[R_hg[sub]])
            for kk in range(4):
                b = nextbank(0, 4)
                for k2 in range(2):
                    k = 2 * kk + k2
                    for sub in range(4):
                        K.mm(lambda pe: pe.transpose(pbb[b][:, k2 * 512 + sub * 128:k2 * 512 + (sub + 1) * 128], hg[:, sub, k * 128:(k + 1) * 128], ident_b.ap),
                             reads=[R_hg[sub], ident_b.res], writes=[PB[b]], signal=(k2 == 1 and sub == 3))
                if ncp % 2 == 0:
                    K.op("act", lambda e: e.copy(out=h2cT.v(2 * kk * 512, [[1, 1024]]), in_=pbb[b][:, 0:1024]), reads=[PB[b]], writes=[h2cT.res])
                else:
                    K.op("dve", lambda e: e.tensor_copy(out=h2cT.v(2 * kk * 512, [[1, 1024]]), in_=pbb[b][:, 0:1024]), reads=[PB[b]], writes=[h2cT.res])
                ncp += 1
            at = actT[nact % 2]; nact += 1
            for jj in range(8):
                bg_ = nextbank(0, 4); bu_ = nextbank(0, 4)
                for k in range(8):
                    K.mm(lambda pe: pe.matmul(pb[bg_][:, :], lhsT=wg[:, k, jj * 128:(jj + 1) * 128], rhs=h2cT[:, k, :], start=(k == 0), stop=(k == 7)),
                         reads=[rg[jj // 4], h2cT.res], writes=[PB[bg_]], signal=(k == 7))
                for k in range(8):
                    K.mm(lambda pe: pe.matmul(pb[bu_][:, :], lhsT=wg[:, k, D + jj * 128:D + (jj + 1) * 128], rhs=h2cT[:, k, :], start=(k == 0), stop=(k == 7)),
                         reads=[rg[2 + jj // 4], h2cT.res], writes=[PB[bu_]], signal=(k == 7))
                gt = gtb[nel[0] % 2]; st_ = stb[nel[0] % 2]; ut = utb[nel[0] % 2]; nel[0] += 1
                K.op("dve", lambda e: e.tensor_scalar(out=gt.ap, in0=pb[bg_][:, :], scalar1=bguT[:, jj, ei:ei + 1], scalar2=7.0, op0=ALU.add, op1=ALU.min),
                     reads=[PB[bg_], bguT.res], writes=[gt.res])
                K.op("act", lambda e: e.activation(out=st_.ap, in_=gt.ap, func=AF.Silu, scale=1.702), reads=[gt.res], writes=[st_.res])
                K.op("dve", lambda e: e.tensor_scalar(out=ut.ap, in0=pb[bu_][:, :], scalar1=bguT1[:, jj, ei:ei + 1], scalar2=-6.0, op0=ALU.add, op1=ALU.max),
                     reads=[PB[bu_], bguT1.res], writes=[ut.res])
                K.op("dve", lambda e: e.scalar_tensor_tensor(out=at[:, jj, :], in0=ut.ap, scalar=8.0, in1=st_.ap, op0=ALU.min, op1=ALU.mult), reads=[ut.res, st_.res], writes=[at.res])
            for sub in range(4):
                for dh in range(2):
                    bo = nextbank(4, 8)
                    for f in range(8):
                        K.mm(lambda pe: pe.matmul(pb[bo][:, :], lhsT=at[:, f, sub * 128:(sub + 1) * 128], rhs=wdn[:, f, dh * 512:(dh + 1) * 512], start=(f == 0), stop=(f == 7)),
                             reads=[at.res, rd[dh]], writes=[PB[bo]], signal=(f == 7))
                    K.op("dve", lambda e: e.tensor_scalar(out=ys[:, sub, dh * 512:(dh + 1) * 512], in0=pb[bo][:, :], scalar1=hg[:, sub, D:D + 64].bitcast(F32)[:, ei:ei + 1], scalar2=float(1.0 / 1.702), op0=ALU.mult, op1=ALU.mult),
                         reads=[PB[bo], R_hg[sub]], writes=[R_ys[sub]])
                K.idma(out=YS.ap()[:, :], out_off=IO(ap=iv(posi[:, sub:sub + 1]), axis=0), in_=ys[:, sub, :], in_off=None, bc=NSLOT - 1,
                       reads=[R_ys[sub], posi.res], writes=[Res()])
            K.guard_end()
            if ei + 1 < n_exp and j < 2:
                for pc in range(3 * j, 3 * j + 3):
                    load_piece(ei + 1, pc)
    K.barrier()
    A.release(m5b)

    yk = A.alloc([128, 4, D], F32, "yk"); R_yk = [Res() for _ in range(4)]
    accb = A.alloc([128, D], F32, "accb")
    bdn = A.alloc([NE, D], F32, "bdn")
    wT = A.alloc([NE, 128], F32, "wT")
    fnb = A.alloc([128, D], F32, "fnb")
    xo = A.alloc([128, D], F32, "xo")
    ss3 = A.alloc([128, NT], F32, "ss3"); r3 = A.alloc([128, NT], F32, "r3"); rstd3 = A.alloc([128, NT], F32, "rstd3")
    K.dma("sp", fnb.ap, dram_ap(fn_d, 0, [[0, 128], [1, D]]), writes=[fnb.res])
    K.dma("sp", bdn.ap, bd_d.ap()[:, :], writes=[bdn.res])
    for c in range(NT):
        b = nextbank()
        K.mm(lambda pe: pe.transpose(pb[b][0:NE, 0:128], wS[:, c, :], ident_f.ap), reads=[wS.res, ident_f.res], writes=[PB[b]])
        K.op("dve", lambda e: e.tensor_copy(out=wT.ap, in_=pb[b][0:NE, 0:128]), reads=[PB[b]], writes=[wT.res])
        K.dma("sp", xo.ap, rows(X1, c, D), writes=[xo.res])
        for k in range(4):
            K.idma(out=yk[:, k, :], out_off=None, in_=YS.ap()[:, :], in_off=IO(ap=iv(poskS[:, c, k:k + 1]), axis=0), bc=NSLOT - 1, reads=[poskS.res], writes=[R_yk[k]])
        for dh in range(2):
            b2 = nextbank()
            K.mm(lambda pe: pe.matmul(pb[b2][:, :], lhsT=wT.ap, rhs=bdn[:, dh * 512:(dh + 1) * 512], start=True, stop=True), reads=[wT.res, bdn.res], writes=[PB[b2]])
            K.op("dve", lambda e: e.tensor_tensor(out=accb[:, dh * 512:(dh + 1) * 512], in0=pb[b2][:, :], in1=yk[:, 0, dh * 512:(dh + 1) * 512], op=ALU.add),
                 reads=[PB[b2], R_yk[0]], writes=[accb.res])
        for k in range(1, 4):
            K.op("dve", lambda e: e.tensor_tensor(out=accb.ap, in0=accb.ap, in1=yk[:, k, :], op=ALU.add), reads=[accb.res, R_yk[k]], writes=[accb.res])
        K.op("dve", lambda e: e.tensor_tensor(out=accb.ap, in0=accb.ap, in1=g2b.ap, op=ALU.mult), reads=[accb.res, g2b.res], writes=[accb.res])
        K.op("dve", lambda e: e.tensor_tensor(out=xo.ap, in0=xo.ap, in1=accb.ap, op=ALU.add), reads=[xo.res, accb.res], writes=[xo.res])
        K.op("act", lambda e: e.activation(out=accb.ap, in_=xo.ap, func=AF.Square, accum_out=ss3[:, c:c + 1]), reads=[xo.res], writes=[accb.res, ss3.res])
        K.op("act", lambda e: e.activation(out=r3[:, c:c + 1], in_=ss3[:, c:c + 1], func=AF.Sqrt, bias=epsT[:, 0:1], scale=1.0 / D),
             reads=[ss3.res, epsT.res], writes=[r3.res])
        K.op("dve", lambda e: e.reciprocal(out=rstd3[:, c:c + 1], in_=r3[:, c:c + 1]), reads=[r3.res], writes=[rstd3.res])
        K.op("dve", lambda e: e.scalar_tensor_tensor(out=xo.ap, in0=xo.ap, scalar=rstd3[:, c:c + 1], in1=fnb.ap, op0=ALU.mult, op1=ALU.mult),
             reads=[xo.res, rstd3.res, fnb.res], writes=[xo.res])
        K.dma("sp", rows(out_d, c, D), xo.ap, reads=[xo.res], writes=[R_OUT])
    K.barrier()
    return nc, dbg, K, A


def host_consts():
    t = np.arange(4096, dtype=np.int64)
    m = (t[:, None] * t[None, :]) % 4096
    ang = 2.0 * np.pi * m.astype(np.float64) / 4096.0
    c = np.cos(ang).astype(np.float32); ns = (-np.sin(ang)).astype(np.float32)

    def lay(a):
        a = a.reshape(32, 128, 16, 256).transpose(2, 1, 0, 3).reshape(16, 128, 32 * 256)
        return np.ascontiguousarray(a).astype(ml_dtypes.bfloat16)
    cc = np.arange(128, dtype=np.int64)
    a2 = 2.0 * np.pi * ((cc[:, None] * cc[None, :]) % 128).astype(np.float64) / 128.0
    cd = np.concatenate([np.cos(a2), np.sin(a2)], axis=1).astype(np.float32).astype(ml_dtypes.bfloat16)
    return lay(c), lay(ns), cd


_CONSTS = None


def make_in_maps(inputs, cores):
    global _CONSTS
    if _CONSTS is None:
        _CONSTS = host_consts()
    ct, nst, cd = _CONSTS
    f = lambda a: np.ascontiguousarray(np.asarray(a, dtype=np.float32))
    shared = {
        "c_ctx": f(inputs["c_ctx"]).reshape(1, D), "w_mod": f(inputs["w_mod"][0]), "b_mod": f(inputs["b_mod"]).reshape(1, 6 * D),
        "norm1_w": f(inputs["norm1_w"]).reshape(1, D), "norm2_w": f(inputs["norm2_w"]).reshape(1, D),
        "w_in": f(inputs["w_in"][0]), "conv_w": f(inputs["conv_w"][0]), "conv_b": f(inputs["conv_b"]).reshape(1, 4096),
        "dt_bias": f(inputs["dt_bias"]).reshape(1, 64), "a_log": f(inputs["a_log"]).reshape(1, 64), "d_skip": f(inputs["d_skip"]).reshape(1, NH),
        "ssd_norm_w": f(inputs["ssd_norm_w"]).reshape(1, DI), "w_ssd_out": f(inputs["w_ssd_out"][0]), "w_four_out": f(inputs["w_four_out"][0]),
        "w_o": f(inputs["w_o"][0]), "w_router": f(inputs["w_router"][0]), "b_router": f(inputs["b_router"]).reshape(1, NE),
        "w_gate_up": f(inputs["w_gate_up"][0]), "b_gate_up": f(inputs["b_gate_up"][0]), "w_down": f(inputs["w_down"][0]),
        "b_down": f(inputs["b_down"][0]), "final_norm_w": f(inputs["final_norm_w"]).reshape(1, D),
        "dft_c": ct, "dft_ns": nst, "cdft": cd,
    }
    maps = []
    for b in cores:
        m = dict(shared)
        m["x"] = f(inputs["x"][b]); m["c"] = f(inputs["c"][b]).reshape(1, D); m["ctx"] = f(inputs["ctx"][b])
        maps.append(m)
    return maps


def kernel(**inputs):
    nc, _, _, _ = build()
    maps = make_in_maps(inputs, list(range(8)))
    res = run_bass_kernel_spmd(nc, maps, core_ids=list(range(8)))
    return np.stack([np.asarray(r["out"], dtype=np.float32) for r in res.results], axis=0)
```
